# Optimizing a Trainium2 kernel written in Bass

```python
import jax, jax.numpy as jnp
from jax import lax
import numpy as np

D_MODEL = 1024
BATCH = 8
SEQ = 4096
DEPTH = 4

N_MIXERS = 2
CONV_KERNEL = 31
N_HEADS = 16
N_KV_HEADS = 4
HEAD_DIM = D_MODEL // N_HEADS
ROT_DIM = HEAD_DIM // 4
ROPE_THETA = 500000.0
WINDOW = 128
BLOCK = 128
D_FF_DENSE = 256 * ((8 * D_MODEL // 3 + 255) // 256)
N_EXPERTS = 8
TOP_K = 2
D_FF_EXPERT = 7 * D_MODEL // 2
EPS = 1e-6
N_CONV_LAYERS = (DEPTH + 1) // 2
N_ATTN_LAYERS = DEPTH // 2

kernel_name = "hybrid_conformer_swa_sink_moe_adaln"


def rms_norm(x, g):
    xf = x.astype(jnp.float32)
    y = xf * lax.rsqrt(jnp.mean(xf * xf, axis=-1, keepdims=True) + EPS)
    return (y * g.astype(jnp.float32)).astype(x.dtype)


def layer_norm(x, g, b):
    xf = x.astype(jnp.float32)
    mu = jnp.mean(xf, axis=-1, keepdims=True)
    var = jnp.mean(jnp.square(xf - mu), axis=-1, keepdims=True)
    y = (xf - mu) * lax.rsqrt(var + EPS)
    return (y * g.astype(jnp.float32) + b.astype(jnp.float32)).astype(x.dtype)


def modulate(h, shift, scale):
    return h * (1 + scale[:, None, :]) + shift[:, None, :]


def conformer_conv(h, w_pw1, b_pw1, w_dw, b_dw, ln_g, ln_b, w_pw2, b_pw2):
    u = h @ w_pw1 + b_pw1
    a, g = jnp.split(u, 2, axis=-1)
    u = a * jax.nn.sigmoid(g)
    u = lax.conv_general_dilated(
        u, w_dw[:, None, :].astype(u.dtype), window_strides=(1,),
        padding=[(CONV_KERNEL - 1, 0)],
        dimension_numbers=('NWC', 'WIO', 'NWC'),
        feature_group_count=D_MODEL) + b_dw
    u = jax.nn.silu(layer_norm(u, ln_g, ln_b))
    return u @ w_pw2 + b_pw2


def rope_tables(positions):
    inv_freq = ROPE_THETA ** (-jnp.arange(0, ROT_DIM, 2, dtype=jnp.float32) / ROT_DIM)
    ang = positions.astype(jnp.float32)[..., None] * inv_freq
    return jnp.cos(ang), jnp.sin(ang)


def apply_partial_rope(x, cos, sin):
    xr = x[..., :ROT_DIM].astype(jnp.float32)
    x1, x2 = jnp.split(xr, 2, axis=-1)
    c = cos[:, :, None, :]
    s = sin[:, :, None, :]
    rot = jnp.concatenate([x1 * c - x2 * s, x2 * c + x1 * s], axis=-1)
    return jnp.concatenate([rot.astype(x.dtype), x[..., ROT_DIM:]], axis=-1)


def swa_sink_attention(h, cos, sin, w_qkv, q_gain, k_gain, sinks, w_o):
    B, S, _ = h.shape
    nb = S // BLOCK
    G = N_HEADS // N_KV_HEADS
    qkv = h @ w_qkv
    nq = N_HEADS * HEAD_DIM
    nk = N_KV_HEADS * HEAD_DIM
    q = qkv[..., :nq].reshape(B, S, N_HEADS, HEAD_DIM)
    k = qkv[..., nq:nq + nk].reshape(B, S, N_KV_HEADS, HEAD_DIM)
    v = qkv[..., nq + nk:].reshape(B, S, N_KV_HEADS, HEAD_DIM)
    q = apply_partial_rope(rms_norm(q, q_gain), cos, sin)
    k = apply_partial_rope(rms_norm(k, k_gain), cos, sin)
    q = q.reshape(B, nb, BLOCK, N_KV_HEADS, G, HEAD_DIM)

    def band(t):
        prev = jnp.pad(t, ((0, 0), (BLOCK, 0), (0, 0), (0, 0)))[:, :S]
        prev = prev.reshape(B, nb, BLOCK, N_KV_HEADS, HEAD_DIM)
        cur = t.reshape(B, nb, BLOCK, N_KV_HEADS, HEAD_DIM)
        return jnp.concatenate([prev, cur], axis=2)

    kb, vb = band(k), band(v)
    s = jnp.einsum('bnqkgd,bnjkd->bnkgqj', q, kb).astype(jnp.float32) * (HEAD_DIM ** -0.5)
    qi = jnp.arange(BLOCK)[:, None]
    kj = jnp.arange(2 * BLOCK)[None, :]
    diff = qi + BLOCK - kj
    in_win = (diff >= 0) & (diff < WINDOW)
    key_pos = jnp.arange(nb)[:, None] * BLOCK - BLOCK + kj
    valid = in_win[None] & (key_pos[:, None, :] >= 0)
    s = jnp.where(valid[None, :, None, None], s, jnp.float32(-1e30))
    sink = sinks.astype(jnp.float32).reshape(N_KV_HEADS, G)[None, None, :, :, None, None]
    m = jnp.maximum(jnp.max(s, axis=-1, keepdims=True), sink)
    p = jnp.exp(s - m)
    p = p / (jnp.sum(p, axis=-1, keepdims=True) + jnp.exp(sink - m))
    o = jnp.einsum('bnkgqj,bnjkd->bnqkgd', p.astype(vb.dtype), vb)
    return o.reshape(B, S, N_HEADS * HEAD_DIM) @ w_o


def swiglu(h, w_gate, w_up, w_down):
    return (jax.nn.silu(h @ w_gate) * (h @ w_up)) @ w_down


def moe_swiglu(h, w_router, b_router, w_gate, w_up, w_down):
    B, S, D = h.shape
    t = h.reshape(B * S, D)
    logits = (t @ w_router).astype(jnp.float32) + b_router.astype(jnp.float32)
    vals, idx = lax.top_k(logits, TOP_K)
    gates = jax.nn.softmax(vals, axis=-1)
    combine = jnp.sum(jax.nn.one_hot(idx, N_EXPERTS, dtype=jnp.float32) * gates[..., None], axis=1)
    out = jnp.zeros_like(t)
    for e in range(N_EXPERTS):
        out = out + combine[:, e:e + 1].astype(t.dtype) * swiglu(t, w_gate[e], w_up[e], w_down[e])
    return out.reshape(B, S, D)


def setup_inputs(seed: int = 0) -> dict:
    key = jax.random.key(seed)
    ks = iter(jax.random.split(key, 40))
    D = D_MODEL
    f32 = jnp.float32

    def nrm(shape, scale):
        return jax.random.normal(next(ks), shape, f32) * scale

    nc, na = N_CONV_LAYERS, N_ATTN_LAYERS
    qkv_out = (N_HEADS + 2 * N_KV_HEADS) * HEAD_DIM
    offsets = jax.random.randint(next(ks), (BATCH, 1), 0, 1024, dtype=jnp.int32)
    positions = offsets + jnp.arange(SEQ, dtype=jnp.int32)[None, :]
    return {
        "x": nrm((BATCH, SEQ, D), 1.0),
        "c": nrm((BATCH, D), 1.0),
        "positions": positions,
        "norm_g": 1.0 + nrm((DEPTH, 2, D), 0.02),
        "w_ada": nrm((DEPTH, D, 6 * D), 0.5 * D ** -0.5),
        "b_ada": nrm((DEPTH, 6 * D), 0.02),
        "conv_w_pw1": nrm((nc, D, 2 * D), D ** -0.5),
        "conv_b_pw1": nrm((nc, 2 * D), 0.02),
        "conv_w_dw": nrm((nc, CONV_KERNEL, D), CONV_KERNEL ** -0.5),
        "conv_b_dw": nrm((nc, D), 0.02),
        "conv_ln_g": 1.0 + nrm((nc, D), 0.02),
        "conv_ln_b": nrm((nc, D), 0.02),
        "conv_w_pw2": nrm((nc, D, D), D ** -0.5),
        "conv_b_pw2": nrm((nc, D), 0.02),
        "attn_w_qkv": nrm((na, D, qkv_out), D ** -0.5),
        "attn_q_gain": 1.0 + nrm((na, HEAD_DIM), 0.02),
        "attn_k_gain": 1.0 + nrm((na, HEAD_DIM), 0.02),
        "attn_sinks": nrm((na, N_HEADS), 1.0),
        "attn_w_o": nrm((na, N_HEADS * HEAD_DIM, D), (N_HEADS * HEAD_DIM) ** -0.5),
        "ffn_w_gate": nrm((nc, D, D_FF_DENSE), D ** -0.5),
        "ffn_w_up": nrm((nc, D, D_FF_DENSE), D ** -0.5),
        "ffn_w_down": nrm((nc, D_FF_DENSE, D), D_FF_DENSE ** -0.5),
        "moe_w_router": nrm((na, D, N_EXPERTS), D ** -0.5),
        "moe_b_router": nrm((na, N_EXPERTS), 0.01),
        "moe_w_gate": nrm((na, N_EXPERTS, D, D_FF_EXPERT), D ** -0.5),
        "moe_w_up": nrm((na, N_EXPERTS, D, D_FF_EXPERT), D ** -0.5),
        "moe_w_down": nrm((na, N_EXPERTS, D_FF_EXPERT, D), D_FF_EXPERT ** -0.5),
    }


def reference(x, c, positions, norm_g, w_ada, b_ada,
              conv_w_pw1, conv_b_pw1, conv_w_dw, conv_b_dw, conv_ln_g, conv_ln_b, conv_w_pw2, conv_b_pw2,
              attn_w_qkv, attn_q_gain, attn_k_gain, attn_sinks, attn_w_o,
              ffn_w_gate, ffn_w_up, ffn_w_down,
              moe_w_router, moe_b_router, moe_w_gate, moe_w_up, moe_w_down):
    cos, sin = rope_tables(positions)
    c_act = jax.nn.silu(c)
    for i in range(DEPTH):
        j = i // 2
        mod = c_act @ w_ada[i] + b_ada[i]
        sh1, sc1, g1, sh2, sc2, g2 = jnp.split(mod, 6, axis=-1)
        h = modulate(rms_norm(x, norm_g[i, 0]), sh1, sc1)
        if i % N_MIXERS == 0:
            y = conformer_conv(h, conv_w_pw1[j], conv_b_pw1[j], conv_w_dw[j], conv_b_dw[j],
                               conv_ln_g[j], conv_ln_b[j], conv_w_pw2[j], conv_b_pw2[j])
        else:
            y = swa_sink_attention(h, cos, sin, attn_w_qkv[j], attn_q_gain[j], attn_k_gain[j],
                                   attn_sinks[j], attn_w_o[j])
        x = x + g1[:, None, :] * y
        h = modulate(rms_norm(x, norm_g[i, 1]), sh2, sc2)
        if i % 2 == 0:
            y = swiglu(h, ffn_w_gate[j], ffn_w_up[j], ffn_w_down[j])
        else:
            y = moe_swiglu(h, moe_w_router[j], moe_b_router[j], moe_w_gate[j], moe_w_up[j], moe_w_down[j])
        x = x + g2[:, None, :] * y
    return x
```

```python
import numpy as np
from contextlib import ExitStack
import concourse.bass as bass
import concourse.mybir as mybir
from concourse.bass_utils import run_bass_kernel_spmd

F32 = mybir.dt.float32
BF16 = mybir.dt.bfloat16
I32 = mybir.dt.int32
AF = mybir.ActivationFunctionType
ALU = mybir.AluOpType
AX = mybir.AxisListType

D = 1024
KC = 8
TT = 512
NB = 4
FD = 2816
NFD = 22
FE = 3584
NFE = 28
NE = 8
CK = 31
HALO = 30
EPS = 1e-6
NEG = -30000.0
NSLOT = 4
NDVE_TAP = 7
SLOT = 4096
ENGS = ["sync", "tensor", "vector", "scalar", "gpsimd"]


class Buf:
    __slots__ = ("name", "w", "r", "rd")

    def __init__(self, name):
        self.name = name
        self.w = None
        self.r = {}
        self.rd = []


class Op:
    __slots__ = ("eng", "fn", "deps", "cnt", "sig", "isdma", "dkey", "dval")

    def __init__(self, eng, fn):
        self.eng = eng
        self.fn = fn
        self.deps = []
        self.cnt = 0
        self.sig = False
        self.isdma = False
        self.dkey = None
        self.dval = 0


class Prog:
    def __init__(self):
        self.q = {e: [] for e in ENGS}
        self.dcnt = {}
        self.nbuf = 0

    def buf(self, name=None):
        self.nbuf += 1
        return Buf(name or ("b%d" % self.nbuf))

    def add(self, eng, fn, reads=(), writes=(), dma_key=None, extra=()):
        op = Op(eng, fn)
        deps = {}

        def dep(d):
            if d is not None and d is not op:
                deps[id(d)] = d

        for b in reads:
            dep(b.w)
        for b in writes:
            w = b.w
            if not (w is not None and w.isdma and dma_key is not None and w.dkey == dma_key):
                dep(w)
            for r in b.r.values():
                dep(r)
            for r in b.rd:
                dep(r)
        for d in extra:
            dep(d)
        op.deps = list(deps.values())
        if fn is None:
            assert not reads and not writes
        if dma_key is not None:
            op.isdma = True
            n = self.dcnt.get(dma_key, 0) + 1
            self.dcnt[dma_key] = n
            op.dkey = dma_key
            op.dval = 16 * n
        for b in reads:
            if op.isdma:
                b.rd.append(op)
            else:
                b.r[eng] = op
        for b in writes:
            b.w = op
            b.r = {}
            b.rd = []
        self.q[eng].append(op)
        return op

    def emit(self, nc):
        for e in ENGS:
            for op in self.q[e]:
                for d in op.deps:
                    if not d.isdma and not (d.eng == "tensor" and e == "tensor"):
                        d.sig = True
        for e in ENGS:
            c = 0
            for op in self.q[e]:
                if op.sig and not op.isdma:
                    c += 1
                    op.cnt = c
        with ExitStack() as st:
            esem = {e: st.enter_context(nc.semaphore("es_" + e)) for e in ENGS}
            dsem = {}
            for i, k in enumerate(self.dcnt):
                dsem[k] = st.enter_context(nc.semaphore("ds%d" % i))
            block = st.enter_context(nc.Block())
            for eng in ENGS:
                ops = self.q[eng]
                if not ops:
                    continue

                def body(e, eng=eng, ops=ops):
                    waited = {}
                    for op in ops:
                        need = {}
                        for d in op.deps:
                            if d.isdma:
                                key = ("d", d.dkey)
                                val = d.dval
                            else:
                                if d.eng == "tensor" and eng == "tensor":
                                    continue
                                key = ("e", d.eng)
                                val = d.cnt
                            if need.get(key, 0) < val:
                                need[key] = val
                        for key, val in need.items():
                            if waited.get(key, 0) >= val:
                                continue
                            sem = dsem[key[1]] if key[0] == "d" else esem[key[1]]
                            e.wait_ge(sem, val)
                            waited[key] = val
                        if op.fn is None:
                            continue
                        ins = op.fn(e)
                        if op.isdma:
                            ins.then_inc(dsem[op.dkey], 16)
                        elif op.sig:
                            ins.then_inc(esem[eng], 1)

                getattr(block, eng)(body)


def make_consts():
    c = np.zeros((128, 1537), np.float32)
    c[:, 0:128] = np.eye(128, dtype=np.float32)
    c[:, 128:256] = 1.0
    bo = np.zeros((128, 128), np.float32)
    bo[:64, :64] = 1.0
    bo[64:, 64:] = 1.0
    c[:, 256:384] = bo
    rp = np.zeros((128, 128), np.float32)
    for hb in (0, 64):
        for m in range(8):
            rp[hb + m + 8, hb + m] = -1.0
            rp[hb + m, hb + m + 8] = 1.0
    c[:, 384:512] = rp
    j = np.arange(128)[:, None]
    q = np.arange(128)[None, :]
    cur = np.where(j <= q, 0.0, NEG).astype(np.float32)
    prev = np.where(j > q, 0.0, NEG).astype(np.float32)
    c[:, 512:1024] = np.tile(cur, (1, 4))
    c[:, 1024:1536] = np.tile(prev, (1, 4))
    inv = (500000.0 ** (-np.arange(0, 16, 2, dtype=np.float32) / 16)).astype(np.float32)
    for p in range(128):
        if p % 64 < 16:
            c[p, 1536] = inv[p % 8]
    return c


def q_chunk_heads(c):
    pair, i = c // 4, c % 4
    return 4 * (2 * pair) + i, 4 * (2 * pair + 1) + i


def build(S, layers, n_cores_dummy=None):
    NT = S // TT
    nc = bass.Bass("TRN2", target_bir_lowering=False, )
    P = Prog()

    def din(name, shape, dt=F32):
        return nc.dram_tensor(name, list(shape), dt, kind="ExternalInput").ap()

    x_d = din("x", [S, D])
    c_d = din("c", [1, D])
    pos_d = din("positions", [1, S], I32)
    cst_d = din("cst", [128, 257])
    cst2_d = din("cst2", [128, 1280])
    norm_g_d = din("norm_g", [4, 2, D])
    w_ada_d = din("w_ada", [4, D, 6 * D])
    b_ada_d = din("b_ada", [4, 6 * D])
    cw_pw1_d = din("conv_w_pw1", [2, D, 2 * D])
    cb_pw1_d = din("conv_b_pw1", [2, 2 * D])
    cw_dw_d = din("conv_w_dw", [2, CK, D])
    cb_dw_d = din("conv_b_dw", [2, D])
    cln_g_d = din("conv_ln_g", [2, D])
    cln_b_d = din("conv_ln_b", [2, D])
    cw_pw2_d = din("conv_w_pw2", [2, D, D])
    cb_pw2_d = din("conv_b_pw2", [2, D])
    a_qkv_d = din("attn_w_qkv", [2, D, 1536])
    a_qg_d = din("attn_q_gain", [2, 64])
    a_kg_d = din("attn_k_gain", [2, 64])
    a_sink_d = din("attn_sinks", [2, 16])
    a_wo_d = din("attn_w_o", [2, D, D])
    f_g_d = din("ffn_w_gate", [2, D, FD])
    f_u_d = din("ffn_w_up", [2, D, FD])
    f_d_d = din("ffn_w_down", [2, FD, D])
    m_r_d = din("moe_w_router", [2, D, NE])
    m_rb_d = din("moe_b_router", [2, NE])
    m_g_d = din("moe_w_gate", [2, NE, D, FE])
    m_u_d = din("moe_w_up", [2, NE, D, FE])
    m_d_d = din("moe_w_down", [2, NE, FE, D])
    y_d = nc.dram_tensor("y", [S, D], F32, kind="ExternalOutput").ap()

    def dscr(name, shape):
        return nc.dram_tensor(name, list(shape), BF16, kind="Internal").ap()

    scr = {}
    for L in layers:
        j = L // 2
        if L % 2 == 0:
            scr[("pw1", j)] = dscr("s_pw1_%d" % j, [4, 128, 8 * 512])
            scr[("pw2", j)] = dscr("s_pw2_%d" % j, [2, 128, 8 * 512])
            scr[("fgu", j)] = dscr("s_fgu_%d" % j, [11, 128, 8 * 512])
            scr[("fd", j)] = dscr("s_fd_%d" % j, [8, 128, NFD * 128])
        else:
            scr[("q", j)] = dscr("s_q_%d" % j, [2, 128, 8 * 512])
            scr[("kv", j)] = dscr("s_kv_%d" % j, [1, 128, 8 * 512])
            scr[("wo", j)] = dscr("s_wo_%d" % j, [2, 128, 8 * 512])
            scr[("mgu", j)] = dscr("s_mgu_%d" % j, [NE, 14, 128, 8 * 512])
            scr[("md", j)] = dscr("s_md_%d" % j, [NE, 8, 128, NFE * 128])

    st = ExitStack()
    with st:
        def T(name, shape, dt=F32):
            return st.enter_context(nc.sbuf_tensor("sb_" + name, list(shape), dt))

        def PS(name, shape):
            return st.enter_context(nc.psum_tensor("ps_" + name, list(shape), F32))

        cst = T("cst", [128, 257])
        cbf = T("cbf", [128, 1536], BF16)
        ident_f = cst[:, 0:128]
        ones_f = cst[:, 128:256]
        invf = cst[:, 256:257]
        ident_b = cbf[:, 0:128]
        ones_b = cbf[:, 128:256]
        bones_b = cbf[:, 256:384]
        rotp_b = cbf[:, 384:512]
        mcur_b = cbf[:, 512:1024]
        mprev_b = cbf[:, 1024:1536]

        NV = 192 + 64 + 32 + 64 + 496 + 8 + 4
        vecT = T("vecT", [128, NV])
        V_BADA, V_NG, V_BPW1, V_BDW, V_LNG, V_LNB, V_BPW2, V_WDW, V_C, V_GAIN = 0, 192, 256, 288, 304, 320, 336, 352, 848, 856
        vrow = [T("vrow0", [128, 128]), T("vrow1", [128, 128])]
        modT = T("modT", [128, 4 * 48])
        coef = T("coef", [128, 4, 3, 8])
        cact2 = T("cact2", [128, 8, 2])
        sinkx = T("sinkx", [128, 2, 16])
        brt = T("brt", [128, 2, 8])
        wr_f = T("wr_f", [128, 2, 8, 8])
        wr_b = T("wr_b", [128, 2, 8, 8], BF16)

        x_sb = T("x_sb", [128, 8, TT])
        tmpA = T("tmpA", [128, 8, TT])
        xio = tmpA[:].rearrange("p c t -> p (c t)").rearrange("p (j d) -> p j d", j=4)
        u32 = T("u32", [128, 8, HALO + TT])
        halo = T("halo", [128, 2, 8, HALO])
        h_bf = T("h_bf", [128, 8, TT], BF16)
        sq_bf = T("sq_bf", [128, 8, TT], BF16)
        q_sb = sq_bf[:].rearrange("p c t -> p (c t)").rearrange("p (a b i q) -> p a b i q", a=2, b=4, i=4)
        o_bf = T("o_bf", [128, 8, TT], BF16)
        act_bf = T("act_bf", [128, NFE, TT], BF16)
        sm = [T("sm%d" % i, [128, TT]) for i in range(6)]
        sil = [T("sil%d" % i, [128, TT]) for i in range(2)]
        tc32 = [T("tc%d" % i, [128, TT]) for i in range(2)]
        qn_bf = T("qn_bf", [128, TT], BF16)
        qn_bf2 = T("qn_bf2", [128, TT], BF16)
        Ctab = T("Ctab", [128, TT])
        Stab = T("Stab", [128, TT])
        pos_i = T("pos_i", [128, TT], I32)
        kz = T("kz", [128, 2, 2, 2, 128 + TT], BF16)
        vaug = T("vaug", [128, 2, 5, 4, 128], BF16)
        pT = [T("pT%d" % i, [128, 2, TT], BF16) for i in range(2)]
        lg = T("lg", [128, 4, 8])
        lg2 = T("lg2", [128, 4, 8])
        eq1 = T("eq1", [128, 4, 8])
        cwt = T("cwt", [128, 4, 8])
        m1 = T("m1", [128, 4])
        m2 = T("m2", [128, 4])
        dg = [T("dg%d" % i, [128, 4, 128]) for i in range(2)]
        wsb = T("wsb", [128, NSLOT * SLOT // 2])
        slots = [wsb[:, i * (SLOT // 2):(i + 1) * (SLOT // 2)].bitcast(BF16) for i in range(NSLOT)]

        psA = PS("psA", [128, 2 * TT])
        psB = PS("psB", [128, 2 * TT])
        psC = [PS("psC0", [128, TT]), PS("psC1", [128, TT])]
        psS = PS("psS", [128, TT])
        psT = PS("psT", [128, TT])

        B = P.buf
        b_cst, b_cbf, b_vecT, b_modT, b_coef, b_cact, b_sink, b_brt, b_wrf, b_wrb = (B() for _ in range(10))
        b_vrow = [B(), B()]
        b_x = [B() for _ in range(8)]
        b_tmpA = [B() for _ in range(8)]
        b_u32 = B()
        b_halo = [B(), B()]
        b_h = [B() for _ in range(8)]
        b_sq = B()
        b_o = [B() for _ in range(8)]
        b_act = [B() for _ in range(NFE)]
        b_sm = [B() for _ in range(6)]
        b_sil = [B(), B()]
        b_tc = [B(), B()]
        b_qn = B()
        b_qn2 = B()
        b_CS = B()
        b_pos = B()
        b_kz = [B(), B()]
        b_va = [B(), B()]
        b_pT = [B(), B()]
        b_lg, b_lg2, b_eq1, b_cwt, b_m1, b_m2 = (B() for _ in range(6))
        b_dg = [B(), B()]
        b_slot = [B() for _ in range(NSLOT)]
        b_A = [B(), B()]
        b_B = [B(), B()]
        b_C = [B(), B()]
        b_S = B()
        b_T = B()
        psAh = [psA[:, 0:TT], psA[:, TT:2 * TT]]
        psBh = [psB[:, 0:TT], psB[:, TT:2 * TT]]

        def MM(out, lhsT, rhs, start, stop, rd, wr):
            return P.add("tensor", lambda e: e.matmul(out, lhsT=lhsT, rhs=rhs, start=start, stop=stop), reads=rd, writes=wr)

        def TR(out, in_, ident, rd, wr):
            return P.add("tensor", lambda e: e.transpose(out=out, in_=in_, identity=ident), reads=rd, writes=wr)

        def ACT(out, in_, func, rd, wr, scale=1.0, bias=None):
            if bias is None:
                return P.add("scalar", lambda e: e.activation(out=out, in_=in_, func=func, scale=scale), reads=rd, writes=wr)
            return P.add("scalar", lambda e: e.activation(out=out, in_=in_, func=func, scale=scale, bias=bias), reads=rd, writes=wr)

        def TS(eng, out, in0, s1, op0, rd, wr, s2=None, op1=None):
            if op1 is None:
                return P.add(eng, lambda e: e.tensor_scalar(out=out, in0=in0, scalar1=s1, scalar2=None, op0=op0), reads=rd, writes=wr)
            return P.add(eng, lambda e: e.tensor_scalar(out=out, in0=in0, scalar1=s1, scalar2=s2, op0=op0, op1=op1), reads=rd, writes=wr)

        def STT(out, in0, scalar, in1, op0, op1, rd, wr):
            return P.add("vector", lambda e: e.scalar_tensor_tensor(out=out, in0=in0, scalar=scalar, in1=in1, op0=op0, op1=op1), reads=rd, writes=wr)

        def TTo(eng, out, in0, in1, op, rd, wr):
            return P.add(eng, lambda e: e.tensor_tensor(out=out, in0=in0, in1=in1, op=op), reads=rd, writes=wr)

        def CP(eng, out, in_, rd, wr):
            if eng == "scalar":
                return P.add(eng, lambda e: e.activation(out=out, in_=in_, func=AF.Copy), reads=rd, writes=wr)
            return P.add(eng, lambda e: e.tensor_copy(out=out, in_=in_), reads=rd, writes=wr)

        def RECIP(out, in_, rd, wr):
            return P.add("vector", lambda e: e.reciprocal(out=out, in_=in_), reads=rd, writes=wr)

        def MSET(eng, ap, val, wr):
            return P.add(eng, lambda e: e.memset(ap, val), writes=wr)

        def DMA(eng, out, in_, rd, wr, key, extra=()):
            return P.add(eng, lambda e: e.dma_start(out=out, in_=in_), reads=rd, writes=wr, dma_key=key, extra=extra)

        DMA("sync", cst[:], cst_d, [], [b_cst], "cst")
        DMA("sync", tmpA[:, 0:3, :].rearrange("p a b -> p (a b)")[:, 0:1280], cst2_d, [], b_tmpA, "cst2")
        CP("vector", cbf[:, 0:256], cst[:, 0:256], [b_cst], [b_cbf])
        CP("vector", cbf[:, 256:1536], tmpA[:, 0:3, :].rearrange("p a b -> p (a b)")[:, 0:1280], b_tmpA, [b_cbf])
        for i in range(2):
            MSET("vector", vrow[i][:], 0.0, [b_vrow[i]])
        MSET("vector", halo[:], 0.0, b_halo)
        MSET("gpsimd", kz[:], 0.0, b_kz)
        MSET("gpsimd", vaug[:], 1.0, b_va)

        vcount = [0]

        def load_vec(src2d, nrows, col0):
            r0 = 0
            while r0 < nrows:
                n = min(128, nrows - r0)
                i = vcount[0] % 2
                vcount[0] += 1
                DMA("sync", vrow[i][0:n, :], src2d[r0:r0 + n, :], [], [b_vrow[i]], ("vrow", i))
                TR(psT[:, 0:128], vrow[i][:], ident_f, [b_vrow[i], b_cst], [b_T])
                CP("vector", vecT[:, col0 + r0:col0 + r0 + n], psT[:, 0:n], [b_T], [b_vecT])
                r0 += n

        load_vec(b_ada_d.rearrange("i (r p) -> (i r) p", p=128), 192, V_BADA)
        load_vec(norm_g_d.rearrange("i s (r p) -> (i s r) p", p=128), 64, V_NG)
        load_vec(cb_pw1_d.rearrange("l (r p) -> (l r) p", p=128), 32, V_BPW1)
        load_vec(cb_dw_d.rearrange("l (r p) -> (l r) p", p=128), 16, V_BDW)
        load_vec(cln_g_d.rearrange("l (r p) -> (l r) p", p=128), 16, V_LNG)
        load_vec(cln_b_d.rearrange("l (r p) -> (l r) p", p=128), 16, V_LNB)
        load_vec(cb_pw2_d.rearrange("l (r p) -> (l r) p", p=128), 16, V_BPW2)
        load_vec(cw_dw_d.rearrange("l k (r p) -> (l k r) p", p=128), 496, V_WDW)
        load_vec(c_d.rearrange("o (r p) -> (o r) p", p=128), 8, V_C)
        i = vcount[0] % 2
        vcount[0] += 1
        for l in range(2):
            for which, gd in enumerate((a_qg_d, a_kg_d)):
                for hf in range(2):
                    DMA("sync", vrow[i][l * 2 + which:l * 2 + which + 1, hf * 64:(hf + 1) * 64], gd[l:l + 1, :], [], [b_vrow[i]], ("vrow", i))
        TR(psT[:, 0:128], vrow[i][:], ident_f, [b_vrow[i], b_cst], [b_T])
        CP("vector", vecT[:, V_GAIN:V_GAIN + 4], psT[:, 0:4], [b_T], [b_vecT])

        for l in range(2):
            DMA("sync", sinkx[:, l, :], a_sink_d[l:l + 1, :].partition_broadcast(128), [], [b_sink], "sink")
            DMA("sync", brt[:, l, :], m_rb_d[l:l + 1, :].partition_broadcast(128), [], [b_brt], "brt")
            DMA("sync", wr_f[:, l, :, :], m_r_d[l].rearrange("(kc p) e -> p kc e", p=128), [], [b_wrf], "wrf")
        ACT(sinkx[:], sinkx[:], AF.Exp, [b_sink], [b_sink])
        CP("vector", wr_b[:], wr_f[:], [b_wrf], [b_wrb])

        ACT(cact2[:, :, 0], vecT[:, V_C:V_C + 8], AF.Silu, [b_vecT], [b_cact])
        CP("vector", cact2[:, :, 1], cact2[:, :, 0], [b_cact], [b_cact])

        stage_w = [(tmpA, b_tmpA), (u32, [b_u32])]
        wk = 0
        for L in layers:
            for blk in range(12):
                tl, tb = stage_w[wk % 2]
                wk += 1
                DMA("sync", tl[:, :, 0:512], w_ada_d[L, :, blk * 512:(blk + 1) * 512].rearrange("(kc p) n -> p kc n", p=128),
                    [], tb, ("wada", wk % 2))
                for cc in range(4):
                    m = blk * 4 + cc
                    for kc in range(KC):
                        MM(psS[:, 2 * m:2 * m + 2], tl[:, kc, cc * 128:(cc + 1) * 128], cact2[:, kc, :], kc == 0, kc == KC - 1,
                           tb + [b_cact], [b_S])
            TTo("vector", modT[:, L * 48:(L + 1) * 48], psS[:, 0:96].rearrange("p (m two) -> p m two", two=2)[:, :, 0],
                vecT[:, V_BADA + L * 48:V_BADA + (L + 1) * 48], ALU.add, [b_S, b_vecT], [b_modT])
            for s in range(2):
                STT(coef[:, L, s, :], modT[:, L * 48 + (3 * s + 1) * 8:L * 48 + (3 * s + 2) * 8], 1.0,
                    vecT[:, V_NG + L * 16 + s * 8:V_NG + L * 16 + (s + 1) * 8], ALU.add, ALU.mult, [b_modT, b_vecT], [b_coef])
            if L % 2 == 0:
                j = L // 2
                TTo("vector", coef[:, L, 2, :], modT[:, L * 48 + 16:L * 48 + 24], vecT[:, V_BPW2 + j * 8:V_BPW2 + (j + 1) * 8],
                    ALU.mult, [b_modT, b_vecT], [b_coef])

        def mod(L, jj, cc):
            return modT[:, L * 48 + jj * 8 + cc:L * 48 + jj * 8 + cc + 1]

        cast_engs = ["vector", "scalar", "gpsimd"]
        pq = []
        cvt = {"k": 0, "stores": []}
        fst = [wsb[:, i * SLOT:i * SLOT + FE] for i in range(2)]
        b_fst = [B() for _ in range(2)]
        bstv = [act_bf[:, 7 * i:7 * i + 7, :].rearrange("p a b -> p (a b)") for i in range(4)]
        b_bst = [B() for _ in range(4)]

        def convert(src_rows, ncols, store_fn):
            k = cvt["k"]
            cvt["k"] += 1
            f, bf = fst[k % 2], b_fst[k % 2]
            g, bg = bstv[k % 4], b_bst[k % 4]
            DMA("sync", f[:, 0:ncols], src_rows, [], [bf], ("fst", k % 2))
            eng = cast_engs[k % 3]
            CP(eng, g[:, 0:ncols], f[:, 0:ncols], [bf], [bg])
            pq.append((k, g, bg, store_fn))
            if len(pq) > 2:
                flush_one()

        def flush_one():
            k, g, bg, store_fn = pq.pop(0)
            for (dst, src) in store_fn(g):
                cvt["stores"].append(DMA("sync", dst, src, [bg], [], ("bst", k % 4)))

        def a_store(sc, rb, W, entries):
            def fn(g):
                out = []
                for (g0, ng, dcol0, scol0, ncols, sstride) in entries:
                    if ng == 1:
                        out.append((sc[g0, :, rb * W + dcol0:rb * W + dcol0 + ncols], g[:, scol0:scol0 + ncols]))
                    else:
                        dst = sc[g0:g0 + ng, :, rb * W + dcol0:rb * W + dcol0 + ncols].rearrange("g p w -> p g w")
                        src = g[:, scol0:scol0 + ng * sstride].rearrange("p (g s) -> p g s", s=sstride)[:, :, 0:ncols]
                        out.append((dst, src))
                return out
            return fn

        for L in layers:
            j = L // 2
            if L % 2 == 0:
                for rb in range(8):
                    rows = slice(rb * 128, (rb + 1) * 128)
                    convert(cw_pw1_d[j, rows, :], 2048, a_store(scr[("pw1", j)], rb, 512,
                            [(0, 4, 0, 0, 256, 256), (0, 4, 256, 1024, 256, 256)]))
                    convert(cw_pw2_d[j, rows, :], 1024, a_store(scr[("pw2", j)], rb, 512, [(0, 2, 0, 0, 512, 512)]))
                    convert(f_g_d[j, rows, :], FD, a_store(scr[("fgu", j)], rb, 512, [(0, 11, 0, 0, 256, 256)]))
                    convert(f_u_d[j, rows, :], FD, a_store(scr[("fgu", j)], rb, 512, [(0, 11, 256, 0, 256, 256)]))
                for rb in range(NFD):
                    rows = slice(rb * 128, (rb + 1) * 128)
                    convert(f_d_d[j, rows, :], 1024, a_store(scr[("fd", j)], rb, 128, [(0, 8, 0, 0, 128, 128)]))
            else:
                for rb in range(8):
                    rows = slice(rb * 128, (rb + 1) * 128)
                    ents = []
                    for c in range(8):
                        ha, hb_ = q_chunk_heads(c)
                        ents.append((c // 4, 1, (c % 4) * 128, ha * 64, 64, 0))
                        ents.append((c // 4, 1, (c % 4) * 128 + 64, hb_ * 64, 64, 0))
                    ents.append((0, 1, 0, 1024, 512, 0))
                    sq_, skv = scr[("q", j)], scr[("kv", j)]

                    def qkv_store(g, rb=rb, ents=ents, sq_=sq_, skv=skv):
                        out = []
                        for (g0, ng, dcol0, scol0, ncols, _s) in ents[:-1]:
                            out.append((sq_[g0, :, rb * 512 + dcol0:rb * 512 + dcol0 + ncols], g[:, scol0:scol0 + ncols]))
                        out.append((skv[0, :, rb * 512:rb * 512 + 512], g[:, 1024:1536]))
                        return out
                    convert(a_qkv_d[j, rows, :], 1536, qkv_store)
                    convert(a_wo_d[j, rows, :], 1024, a_store(scr[("wo", j)], rb, 512, [(0, 2, 0, 0, 512, 512)]))
        while pq:
            flush_one()
        P.add("sync", None, extra=cvt["stores"])
        ws = {"k": 0}

        def wload(src, n):
            i = ws["k"] % NSLOT
            ws["k"] += 1
            DMA("sync", slots[i][:, 0:n], src, [], [b_slot[i]], ("ws", i))
            return slots[i], b_slot[i]

        hs = [tmpA[:, 0:4, :].rearrange("p a b -> p (a b)"), tmpA[:, 4:8, :].rearrange("p a b -> p (a b)")]
        b_hs = [b_tmpA[0:4], b_tmpA[4:8]]
        cast_rr = [0]
        inl_stores = []
        pend_st = []

        def flush_st():
            dst, slot, n, bs, i = pend_st.pop(0)
            inl_stores.append(DMA("sync", dst, slot[:, 0:n], [bs], [], ("wst", i)))

        def issue_conv(kind, j, ex, idx):
            i = ws["k"] % NSLOT
            ws["k"] += 1
            slot, bs = slots[i], b_slot[i]
            if kind == "gu":
                sv = slot[:].rearrange("p (kc n) -> p kc n", kc=8)
                for h, (wd, col0) in enumerate(((m_g_d, 0), (m_u_d, 256))):
                    hv = hs[h].rearrange("p (kc n) -> p kc n", kc=8)
                    DMA("sync", hv, wd[j, ex, :, idx * 256:(idx + 1) * 256].rearrange("(kc p) n -> p kc n", p=128), [], b_hs[h], ("hs", h))
                    eng = cast_engs[cast_rr[0] % 3]
                    cast_rr[0] += 1
                    CP(eng, sv[:, :, col0:col0 + 256], hv, b_hs[h], [bs])
                n = 4096
                dst = scr[("mgu", j)][ex][idx]
            else:
                src = m_d_d[j, ex, :, idx * 128:(idx + 1) * 128].rearrange("(f p) n -> p f n", p=128)
                for h, (f0, f1) in enumerate(((0, 16), (16, NFE))):
                    hv = hs[h][:, 0:(f1 - f0) * 128].rearrange("p (f n) -> p f n", n=128)
                    DMA("sync", hv, src[:, f0:f1, :], [], b_hs[h], ("hs", h))
                    eng = cast_engs[cast_rr[0] % 3]
                    cast_rr[0] += 1
                    CP(eng, slot[:, f0 * 128:f1 * 128].rearrange("p (f n) -> p f n", n=128), hv, b_hs[h], [bs])
                n = NFE * 128
                dst = scr[("md", j)][ex][idx]
            pend_st.append((dst, slot, n, bs, i))
            if len(pend_st) > 1:
                flush_st()
            return slot, bs

        def rmsnorm_mod(L, s):
            ACT(sq_bf[:], x_sb[:], AF.Square, b_x, [b_sq])
            for c in range(KC):
                MM(psS[:], ones_b, sq_bf[:, c, :], c == 0, c == KC - 1, [b_sq, b_cbf], [b_S])
            ACT(sm[0][:], psS[:], AF.Ln, [b_S], [b_sm[0]], scale=1.0 / D, bias=EPS)
            ACT(sm[0][:], sm[0][:], AF.Exp, [b_sm[0]], [b_sm[0]], scale=-0.5)
            for c in range(KC):
                STT(tmpA[:, c, :], x_sb[:, c, :], coef[:, L, s, c:c + 1], sm[0][:], ALU.mult, ALU.mult,
                    [b_x[c], b_coef, b_sm[0]], [b_tmpA[c]])
            for c in range(KC):
                ACT(h_bf[:, c, :], tmpA[:, c, :], AF.Identity, [b_tmpA[c], b_modT], [b_h[c]], bias=mod(L, 3 * s, c))

        def evac_residual(ps, bps, L, jj, c, eng_add="gpsimd", extra_scale=None, ti=0):
            if extra_scale is None:
                STT(x_sb[:, c, :], ps, mod(L, jj, c), x_sb[:, c, :], ALU.mult, ALU.add, [bps, b_modT, b_x[c]], [b_x[c]])
            else:
                es, bes = extra_scale
                STT(tc32[ti][:], ps, mod(L, jj, c), es, ALU.mult, ALU.mult, [bps, b_modT, bes], [b_tc[ti]])
                TTo("gpsimd", x_sb[:, c, :], x_sb[:, c, :], tc32[ti][:], ALU.add, [b_x[c], b_tc[ti]], [b_x[c]])

        def conv_layer(L, t):
            j = L // 2
            rmsnorm_mod(L, 0)
            CP("gpsimd", u32[:, :, 0:HALO], halo[:, j, :, :], [b_halo[j]], [b_u32])
            k = 0
            for g in range(4):
                w, bw = wload(scr[("pw1", j)][g], 4096)
                wv = w[:].rearrange("p (kc n) -> p kc n", kc=8)
                for cc in range(2):
                    c = 2 * g + cc
                    pa, bpa = psAh[k % 2], b_A[k % 2]
                    pg, bpg = psBh[k % 2], b_B[k % 2]
                    for kc in range(KC):
                        MM(pa, wv[:, kc, cc * 128:(cc + 1) * 128], h_bf[:, kc, :], kc == 0, kc == KC - 1, [bw, b_h[kc]], [bpa])
                    for kc in range(KC):
                        MM(pg, wv[:, kc, 256 + cc * 128:256 + (cc + 1) * 128], h_bf[:, kc, :], kc == 0, kc == KC - 1, [bw, b_h[kc]], [bpg])
                    ACT(sil[k % 2][:], pg, AF.Sigmoid, [bpg, b_vecT], [b_sil[k % 2]],
                        bias=vecT[:, V_BPW1 + j * 16 + 8 + c:V_BPW1 + j * 16 + 8 + c + 1])
                    STT(u32[:, c, HALO:HALO + TT], pa, vecT[:, V_BPW1 + j * 16 + c:V_BPW1 + j * 16 + c + 1], sil[k % 2][:],
                        ALU.add, ALU.mult, [bpa, b_vecT, b_sil[k % 2]], [b_u32])
                    k += 1
            npr = 0
            for kk in range(CK):
                for c in range(KC):
                    wcol = vecT[:, V_WDW + j * 248 + kk * 8 + c:V_WDW + j * 248 + kk * 8 + c + 1]
                    if c >= NDVE_TAP:
                        if kk == 0:
                            P.add("scalar", lambda e, c=c, wcol=wcol: e.activation(
                                out=tmpA[:, c, :], in_=u32[:, c, 0:TT], func=AF.Identity, scale=wcol,
                                bias=vecT[:, V_BDW + j * 8 + c:V_BDW + j * 8 + c + 1]), reads=[b_u32, b_vecT], writes=[b_tmpA[c]])
                        else:
                            pb, bpb = sil[npr % 2], b_sil[npr % 2]
                            npr += 1
                            P.add("scalar", lambda e, c=c, wcol=wcol, kk=kk, pb=pb: e.activation(
                                out=pb[:], in_=u32[:, c, kk:kk + TT], func=AF.Identity, scale=wcol), reads=[b_u32, b_vecT], writes=[bpb])
                            TTo("gpsimd", tmpA[:, c, :], tmpA[:, c, :], pb[:], ALU.add, [b_tmpA[c], bpb], [b_tmpA[c]])
                        continue
                    if kk == 0:
                        TS("vector", tmpA[:, c, :], u32[:, c, 0:TT], wcol, ALU.mult, [b_u32, b_vecT], [b_tmpA[c]],
                           s2=vecT[:, V_BDW + j * 8 + c:V_BDW + j * 8 + c + 1], op1=ALU.add)
                    else:
                        STT(tmpA[:, c, :], u32[:, c, kk:kk + TT], wcol, tmpA[:, c, :], ALU.mult, ALU.add,
                            [b_u32, b_vecT, b_tmpA[c]], [b_tmpA[c]])
            CP("gpsimd", halo[:, j, :, :], u32[:, :, TT:TT + HALO], [b_u32], [b_halo[j]])
            CP("scalar", h_bf[:], tmpA[:], b_tmpA, b_h)
            ACT(sq_bf[:], tmpA[:], AF.Square, b_tmpA, [b_sq])
            for c in range(KC):
                MM(psS[:], ones_b, h_bf[:, c, :], c == 0, c == KC - 1, [b_h[c], b_cbf], [b_S])
            for c in range(KC):
                MM(psT[:], ones_b, sq_bf[:, c, :], c == 0, c == KC - 1, [b_sq, b_cbf], [b_T])
            TS("vector", sm[1][:], psS[:], 1.0 / D, ALU.mult, [b_S], [b_sm[1]])
            TTo("vector", sm[2][:], sm[1][:], sm[1][:], ALU.mult, [b_sm[1]], [b_sm[2]])
            STT(sm[2][:], psT[:], 1.0 / D, sm[2][:], ALU.mult, ALU.subtract, [b_T, b_sm[2]], [b_sm[2]])
            TS("vector", sm[2][:], sm[2][:], 0.0, ALU.max, [b_sm[2]], [b_sm[2]])
            ACT(sm[2][:], sm[2][:], AF.Ln, [b_sm[2]], [b_sm[2]], bias=EPS)
            ACT(sm[2][:], sm[2][:], AF.Exp, [b_sm[2]], [b_sm[2]], scale=-0.5)
            for c in range(KC):
                TTo("vector", tmpA[:, c, :], tmpA[:, c, :], sm[1][:], ALU.subtract, [b_tmpA[c], b_sm[1]], [b_tmpA[c]])
                TTo("vector", tmpA[:, c, :], tmpA[:, c, :], sm[2][:], ALU.mult, [b_tmpA[c], b_sm[2]], [b_tmpA[c]])
            for c in range(KC):
                P.add("scalar", lambda e, c=c: e.activation(
                    out=o_bf[:, c, :], in_=tmpA[:, c, :], func=AF.Silu,
                    scale=vecT[:, V_LNG + j * 8 + c:V_LNG + j * 8 + c + 1],
                    bias=vecT[:, V_LNB + j * 8 + c:V_LNB + j * 8 + c + 1]), reads=[b_tmpA[c], b_vecT], writes=[b_o[c]])
            k = 0
            for g in range(2):
                w, bw = wload(scr[("pw2", j)][g], 4096)
                wv = w[:].rearrange("p (kc n) -> p kc n", kc=8)
                for cc in range(4):
                    c = 4 * g + cc
                    pc, bpc = psC[k % 2], b_C[k % 2]
                    for kc in range(KC):
                        MM(pc[:], wv[:, kc, cc * 128:(cc + 1) * 128], o_bf[:, kc, :], kc == 0, kc == KC - 1, [bw, b_o[kc]], [bpc])
                    evac_residual(pc[:], bpc, L, 2, c)
                    TS("gpsimd", x_sb[:, c, :], x_sb[:, c, :], coef[:, L, 2, c:c + 1], ALU.add, [b_x[c], b_coef], [b_x[c]])
                    k += 1

        def swiglu(L, j, gu_scr, d_scr, nf, jj, extra_scale=None):
            k = 0
            for g in range(nf // 2):
                w, bw = wload(gu_scr[g], 4096)
                wv = w[:].rearrange("p (kc n) -> p kc n", kc=8)
                for cc in range(2):
                    f = 2 * g + cc
                    pg, bpg = psAh[k % 2], b_A[k % 2]
                    pu, bpu = psBh[k % 2], b_B[k % 2]
                    for kc in range(KC):
                        MM(pg, wv[:, kc, cc * 128:(cc + 1) * 128], h_bf[:, kc, :], kc == 0, kc == KC - 1, [bw, b_h[kc]], [bpg])
                    for kc in range(KC):
                        MM(pu, wv[:, kc, 256 + cc * 128:256 + (cc + 1) * 128], h_bf[:, kc, :], kc == 0, kc == KC - 1, [bw, b_h[kc]], [bpu])
                    ACT(sil[k % 2][:], pg, AF.Silu, [bpg], [b_sil[k % 2]])
                    TTo("vector", act_bf[:, f, :], pu, sil[k % 2][:], ALU.mult, [bpu, b_sil[k % 2]], [b_act[f]])
                    k += 1
            for c in range(KC):
                w, bw = wload(d_scr[c], nf * 128)
                wv = w[:, 0:nf * 128].rearrange("p (f n) -> p f n", n=128)
                pc, bpc = psC[c % 2], b_C[c % 2]
                for f in range(nf):
                    MM(pc[:], wv[:, f, :], act_bf[:, f, :], f == 0, f == nf - 1, [bw, b_act[f]], [bpc])
                evac_residual(pc[:], bpc, L, jj, c, extra_scale=extra_scale, ti=c % 2)

        def rope_tables(t):
            t0 = t * TT
            DMA("sync", pos_i[:], pos_d[:, t0:t0 + TT].partition_broadcast(128), [], [b_pos], "pos")
            CP("vector", sm[3][:], pos_i[:], [b_pos], [b_sm[3]])
            TS("vector", sm[3][:], sm[3][:], invf, ALU.mult, [b_sm[3], b_cst], [b_sm[3]])
            M = 12582912.0
            HI = 6.28125
            LO = 2.0 * np.pi - 6.28125
            for which, tab in ((0, Stab), (1, Ctab)):
                src = sm[3]
                if which == 1:
                    TS("vector", sm[4][:], sm[3][:], float(np.pi / 2), ALU.add, [b_sm[3]], [b_sm[4]])
                    src = sm[4]
                bsrc = b_sm[3] if which == 0 else b_sm[4]
                TS("vector", sm[5][:], src[:], float(1.0 / (2 * np.pi)), ALU.mult, [bsrc], [b_sm[5]])
                TS("vector", sm[5][:], sm[5][:], M, ALU.add, [b_sm[5]], [b_sm[5]], s2=M, op1=ALU.subtract)
                STT(tab[:], sm[5][:], -HI, src[:], ALU.mult, ALU.add, [b_sm[5], bsrc], [b_CS])
                STT(tab[:], sm[5][:], -float(LO), tab[:], ALU.mult, ALU.add, [b_sm[5], b_CS], [b_CS])
                TS("vector", tab[:], tab[:], 3.141592, ALU.min, [b_CS], [b_CS], s2=-3.141592, op1=ALU.max)
                ACT(tab[:], tab[:], AF.Sin, [b_CS], [b_CS])

        def qk_post(ps, bps, gcol, out_fn, par):
            if par == 0:
                s_r, b_r, s_q, b_q, s_s, b_s, qb, bqb, pst, bpst = sm[1], b_sm[1], sm[2], b_sm[2], sm[4], b_sm[4], qn_bf, b_qn, psT, b_T
            else:
                s_r, b_r, s_q, b_q, s_s, b_s, qb, bqb, pst, bpst = sm[0], b_sm[0], sm[3], b_sm[3], sm[5], b_sm[5], qn_bf2, b_qn2, psS, b_S
            ACT(qb[:], ps, AF.Square, [bps], [bqb])
            MM(pst[:], bones_b, qb[:], True, True, [bqb, b_cbf], [bpst])
            ACT(s_r[:], pst[:], AF.Ln, [bpst], [b_r], scale=1.0 / 64, bias=EPS)
            ACT(s_r[:], s_r[:], AF.Exp, [b_r], [b_r], scale=-0.5)
            STT(s_q[:], ps, vecT[:, gcol:gcol + 1], s_r[:], ALU.mult, ALU.mult, [bps, b_vecT, b_r], [b_q])
            CP("scalar", qb[:], s_q[:], [b_q], [bqb])
            MM(pst[:], rotp_b, qb[:], True, True, [bqb, b_cbf], [bpst])
            TTo("vector", s_q[:], s_q[:], Ctab[:], ALU.mult, [b_q, b_CS], [b_q])
            TTo("vector", s_s[:], pst[:], Stab[:], ALU.mult, [bpst, b_CS], [b_s])
            out_fn(s_q, b_q, s_s, b_s)

        def attn_layer(L, t):
            j = L // 2
            rmsnorm_mod(L, 0)
            k = 0
            for g in range(2):
                w, bw = wload(scr[("q", j)][g], 4096)
                wv = w[:].rearrange("p (kc n) -> p kc n", kc=8)
                for cc in range(4):
                    c = 4 * g + cc
                    pair, ii = c // 4, c % 4
                    pq_, bpq = psC[k % 2], b_C[k % 2]
                    for kc in range(KC):
                        MM(pq_[:], wv[:, kc, cc * 128:(cc + 1) * 128], h_bf[:, kc, :], kc == 0, kc == KC - 1, [bw, b_h[kc]], [bpq])

                    def outq(sq_, bq_, ss_, bs_, pair=pair, ii=ii):
                        TTo("vector", q_sb[:, pair, :, ii, :], sq_[:].rearrange("p (b q) -> p b q", b=4),
                            ss_[:].rearrange("p (b q) -> p b q", b=4), ALU.add, [bq_, bs_], [b_sq])
                    qk_post(pq_[:], bpq, V_GAIN + j * 2 + 0, outq, k % 2)
                    k += 1
            w, bw = wload(scr[("kv", j)][0], 4096)
            wv = w[:].rearrange("p (kc n) -> p kc n", kc=8)
            for pair in range(2):
                pk, bpk = psC[pair % 2], b_C[pair % 2]
                for kc in range(KC):
                    MM(pk[:], wv[:, kc, pair * 128:(pair + 1) * 128], h_bf[:, kc, :], kc == 0, kc == KC - 1, [bw, b_h[kc]], [bpk])

                def outk(sq_, bq_, ss_, bs_, pair=pair):
                    for hh in range(2):
                        TTo("vector", kz[hh * 64:(hh + 1) * 64, j, pair, hh, 128:128 + TT], sq_[hh * 64:(hh + 1) * 64, :],
                            ss_[hh * 64:(hh + 1) * 64, :], ALU.add, [bq_, bs_], [b_kz[j]])
                qk_post(pk[:], bpk, V_GAIN + j * 2 + 1, outk, pair % 2)
            for blk in range(NB):
                pv, bpv = psAh[blk % 2], b_A[blk % 2]
                for kc in range(KC):
                    MM(pv[:, 0:256], h_bf[:, kc, blk * 128:(blk + 1) * 128], wv[:, kc, 256:512], kc == 0, kc == KC - 1, [bw, b_h[kc]], [bpv])
                CP("scalar", vaug[:, j, 1 + blk, :, 0:64], pv[:, 0:256].rearrange("p (g d) -> p g d", g=4), [bpv], [b_va[j]])
            k = 0
            for blk in range(NB):
                first = (t == 0 and blk == 0)
                for pair in range(2):
                    for hh in range(2):
                        g = 2 * pair + hh
                        pss, bps2 = (psA, b_A) if k % 2 == 0 else (psB, b_B)
                        ptile, bpt = pT[k % 2], b_pT[k % 2]
                        rhs_q = q_sb[:, pair, blk, :, :].rearrange("p i q -> p (i q)")
                        kbs = [1] if first else [0, 1]
                        for kb in kbs:
                            col0 = blk * 128 + kb * 128
                            MM(pss[:, kb * TT:(kb + 1) * TT], kz[:, j, pair, hh, col0:col0 + 128], rhs_q, True, False,
                               [b_kz[j], b_sq], [bps2[kb]])
                            MM(pss[:, kb * TT:(kb + 1) * TT], ident_b, (mprev_b if kb == 0 else mcur_b), False, True,
                               [b_cbf], [bps2[kb]])
                        if first:
                            ACT(ptile[:, 1, :], pss[:, TT:2 * TT], AF.Exp, [bps2[1]], [bpt], scale=0.125)
                        else:
                            ACT(ptile[:].rearrange("p a b -> p (a b)"), pss[:], AF.Exp, [bps2[0], bps2[1]], [bpt], scale=0.125)
                        po, bpo = psC[k % 2], b_C[k % 2]
                        for n_, kb in enumerate(kbs):
                            MM(po[:], vaug[:, j, blk + kb, g, :], ptile[:, kb, :], n_ == 0, n_ == len(kbs) - 1, [b_va[j], bpt], [bpo])
                        den, b_den = (sm[1], b_sm[1]) if k % 2 == 0 else (sm[3], b_sm[3])
                        for ii in range(4):
                            h = 4 * g + ii
                            TS("vector", den[64:128, ii * 128:(ii + 1) * 128], po[64:128, ii * 128:(ii + 1) * 128],
                               sinkx[64:128, j, h:h + 1], ALU.add, [bpo, b_sink], [b_den])
                        ACT(den[64:128, :], den[64:128, :], AF.Ln, [b_den], [b_den])
                        ACT(den[64:128, :], den[64:128, :], AF.Exp, [b_den], [b_den], scale=-1.0)
                        for ii in range(4):
                            h = 4 * g + ii
                            ch, hf = h // 2, h % 2
                            TTo("vector", o_bf[hf * 64:(hf + 1) * 64, ch, blk * 128:(blk + 1) * 128],
                                po[0:64, ii * 128:(ii + 1) * 128], den[64:128, ii * 128:(ii + 1) * 128], ALU.mult,
                                [bpo, b_den], [b_o[ch]])
                        k += 1
            CP("gpsimd", kz[:, j, :, :, 0:128], kz[:, j, :, :, TT:TT + 128], [b_kz[j]], [b_kz[j]])
            CP("gpsimd", vaug[:, j, 0, :, 0:64], vaug[:, j, 4, :, 0:64], [b_va[j]], [b_va[j]])
            k = 0
            for g in range(2):
                w, bw = wload(scr[("wo", j)][g], 4096)
                wv = w[:].rearrange("p (kc n) -> p kc n", kc=8)
                for cc in range(4):
                    c = 4 * g + cc
                    pc, bpc = psC[k % 2], b_C[k % 2]
                    for kc in range(KC):
                        MM(pc[:], wv[:, kc, cc * 128:(cc + 1) * 128], o_bf[:, kc, :], kc == 0, kc == KC - 1, [bw, b_o[kc]], [bpc])
                    evac_residual(pc[:], bpc, L, 2, c)
                    k += 1

        def moe_layer(L, t):
            j = L // 2
            rmsnorm_mod(L, 1)
            for blk in range(NB):
                for kc in range(KC):
                    MM(psT[:, blk * 8:(blk + 1) * 8], h_bf[:, kc, blk * 128:(blk + 1) * 128], wr_b[:, j, kc, :], kc == 0, kc == KC - 1,
                       [b_h[kc], b_wrb], [b_T])
            for blk in range(NB):
                TTo("vector", lg[:, blk, :], psT[:, blk * 8:(blk + 1) * 8], brt[:, j, :], ALU.add, [b_T, b_brt], [b_lg])
            P.add("vector", lambda e: e.tensor_reduce(out=m1[:], in_=lg[:], axis=AX.X, op=ALU.max), reads=[b_lg], writes=[b_m1])
            for blk in range(NB):
                TS("vector", eq1[:, blk, :], lg[:, blk, :], m1[:, blk:blk + 1], ALU.is_equal, [b_lg, b_m1], [b_eq1])
            STT(lg2[:].rearrange("p a b -> p (a b)"), eq1[:].rearrange("p a b -> p (a b)"), -1e30, lg[:].rearrange("p a b -> p (a b)"),
                ALU.mult, ALU.add, [b_eq1, b_lg], [b_lg2])
            P.add("vector", lambda e: e.tensor_reduce(out=m2[:], in_=lg2[:], axis=AX.X, op=ALU.max), reads=[b_lg2], writes=[b_m2])
            for blk in range(NB):
                TS("vector", eq1[:, blk, :], lg[:, blk, :], m2[:, blk:blk + 1], ALU.is_ge, [b_lg, b_m2, b_eq1], [b_eq1])
            TS("vector", m1[:], m1[:], -1.0, ALU.mult, [b_m1], [b_m1])
            for blk in range(NB):
                ACT(lg2[:, blk, :], lg[:, blk, :], AF.Exp, [b_lg, b_m1, b_lg2], [b_lg2], bias=m1[:, blk:blk + 1])
            TTo("vector", lg2[:], lg2[:], eq1[:], ALU.mult, [b_lg2, b_eq1], [b_lg2])
            P.add("vector", lambda e: e.tensor_reduce(out=m2[:], in_=lg2[:], axis=AX.X, op=ALU.add), reads=[b_lg2], writes=[b_m2])
            RECIP(m2[:], m2[:], [b_m2], [b_m2])
            for blk in range(NB):
                TS("vector", cwt[:, blk, :], lg2[:, blk, :], m2[:, blk:blk + 1], ALU.mult, [b_lg2, b_m2], [b_cwt])
            for ex in range(NE):
                d_, bd = dg[ex % 2], b_dg[ex % 2]
                for blk in range(NB):
                    TS("vector", d_[:, blk, :], ident_f, cwt[:, blk, ex:ex + 1], ALU.mult, [b_cst, b_cwt], [bd])
                for blk in range(NB):
                    MM(psS[:, blk * 128:(blk + 1) * 128], ones_f, d_[:, blk, :], True, True, [b_cst, bd], [b_S])
                CP("scalar", u32[:, ex, 0:TT], psS[:], [b_S], [b_u32])
            reqs = []
            for ex in range(NE):
                for g in range(NFE // 2):
                    reqs.append(("gu", ex, g))
                for c in range(KC):
                    reqs.append(("d", ex, c))

            def issue(r):
                kind, ex, idx = r
                if t == 0:
                    return issue_conv(kind, j, ex, idx)
                if kind == "gu":
                    return wload(scr[("mgu", j)][ex][idx], 4096)
                return wload(scr[("md", j)][ex][idx], NFE * 128)

            kcnt = 0
            nxt = issue(reqs[0])
            for ri, r in enumerate(reqs):
                w, bw = nxt
                if ri + 1 < len(reqs):
                    nxt = issue(reqs[ri + 1])
                kind, ex, idx = r
                if kind == "gu":
                    wv = w[:].rearrange("p (kc n) -> p kc n", kc=8)
                    for cc in range(2):
                        f = 2 * idx + cc
                        pg, bpg = psAh[kcnt % 2], b_A[kcnt % 2]
                        pu, bpu = psBh[kcnt % 2], b_B[kcnt % 2]
                        for kc in range(KC):
                            MM(pg, wv[:, kc, cc * 128:(cc + 1) * 128], h_bf[:, kc, :], kc == 0, kc == KC - 1, [bw, b_h[kc]], [bpg])
                        for kc in range(KC):
                            MM(pu, wv[:, kc, 256 + cc * 128:256 + (cc + 1) * 128], h_bf[:, kc, :], kc == 0, kc == KC - 1, [bw, b_h[kc]], [bpu])
                        ACT(sil[kcnt % 2][:], pg, AF.Silu, [bpg], [b_sil[kcnt % 2]])
                        TTo("vector", act_bf[:, f, :], pu, sil[kcnt % 2][:], ALU.mult, [bpu, b_sil[kcnt % 2]], [b_act[f]])
                        kcnt += 1
                else:
                    c = idx
                    wv = w[:, 0:NFE * 128].rearrange("p (f n) -> p f n", n=128)
                    pc, bpc = psC[c % 2], b_C[c % 2]
                    for f in range(NFE):
                        MM(pc[:], wv[:, f, :], act_bf[:, f, :], f == 0, f == NFE - 1, [bw, b_act[f]], [bpc])
                    evac_residual(pc[:], bpc, L, 5, c, extra_scale=(u32[:, ex, 0:TT], b_u32), ti=c % 2)
            if t == 0:
                while pend_st:
                    flush_st()
                P.add("sync", None, extra=list(inl_stores))

        need_rope = any(L % 2 == 1 for L in layers)
        for t in range(NT):
            t0 = t * TT
            DMA("scalar", xio, x_d[t0:t0 + TT, :].rearrange("(j p) d -> p j d", p=128), [], b_tmpA, "xin")
            for c in range(KC):
                for jb in range(4):
                    TR(psT[:, jb * 128:(jb + 1) * 128], xio[:, jb, c * 128:(c + 1) * 128], ident_f, b_tmpA + [b_cst], [b_T])
                CP("vector" if c % 2 == 0 else "scalar", x_sb[:, c, :], psT[:], [b_T], [b_x[c]])
            if need_rope:
                rope_tables(t)
            for L in layers:
                if L % 2 == 0:
                    conv_layer(L, t)
                    rmsnorm_mod(L, 1)
                    swiglu(L, L // 2, scr[("fgu", L // 2)], scr[("fd", L // 2)], NFD, 5)
                else:
                    attn_layer(L, t)
                    moe_layer(L, t)
            for jb in range(4):
                for c in range(KC):
                    TR(psT[:, (c % 4) * 128:(c % 4 + 1) * 128], x_sb[:, c, jb * 128:(jb + 1) * 128], ident_f, [b_x[c], b_cst], [b_T])
                    if c % 4 == 3:
                        cb = c // 4
                        CP("vector" if cb == 0 else "scalar", xio[:, jb, cb * 512:(cb + 1) * 512], psT[:], [b_T], b_tmpA)
            last_store = DMA("scalar", y_d[t0:t0 + TT, :].rearrange("(j p) d -> p j d", p=128), xio, b_tmpA, [], "xout")
        P.add("scalar", None, extra=[last_store])
        P.emit(nc)
    return nc


_W_NAMES = ["norm_g", "w_ada", "b_ada", "conv_w_pw1", "conv_b_pw1", "conv_w_dw", "conv_b_dw", "conv_ln_g", "conv_ln_b",
            "conv_w_pw2", "conv_b_pw2", "attn_w_qkv", "attn_q_gain", "attn_k_gain", "attn_sinks", "attn_w_o",
            "ffn_w_gate", "ffn_w_up", "ffn_w_down", "moe_w_router", "moe_b_router", "moe_w_gate", "moe_w_up", "moe_w_down"]
_CACHE = {}


def run_layers(x, c, positions, weights, layers, runner=None):
    Bn, S, _ = x.shape
    key = (S, tuple(layers))
    if key not in _CACHE:
        _CACHE[key] = build(S, list(layers))
    nc = _CACHE[key]
    cfull = make_consts()
    cst = np.ascontiguousarray(np.concatenate([cfull[:, 0:256], cfull[:, 1536:1537]], axis=1))
    cst2 = np.ascontiguousarray(cfull[:, 256:1536])
    in_maps = []
    for b in range(Bn):
        m = {"x": np.ascontiguousarray(x[b]), "c": np.ascontiguousarray(c[b:b + 1]),
             "positions": np.ascontiguousarray(positions[b:b + 1]).astype(np.int32), "cst": cst, "cst2": cst2}
        for n in _W_NAMES:
            m[n] = weights[n]
        in_maps.append(m)
    if runner is None:
        res = run_bass_kernel_spmd(nc, in_maps, core_ids=list(range(Bn)))
    else:
        res = runner(nc, in_maps)
    return np.stack([res.results[b]["y"] for b in range(Bn)], axis=0)


LAUNCH_GROUPS = [[0, 1, 2, 3]]


def kernel(**inputs):
    x = np.asarray(inputs["x"], dtype=np.float32)
    c = np.asarray(inputs["c"], dtype=np.float32)
    positions = np.asarray(inputs["positions"])
    weights = {n: np.ascontiguousarray(np.asarray(inputs[n], dtype=np.float32)) for n in _W_NAMES}
    for grp in LAUNCH_GROUPS:
        x = run_layers(x, c, positions, weights, grp)
    return x.astype(np.float32)
```

```python
import numpy as np
from contextlib import ExitStack
import concourse.bass as bass
import concourse.mybir as mybir
from concourse.bass_utils import run_bass_kernel_spmd

F32 = mybir.dt.float32
BF16 = mybir.dt.bfloat16
I32 = mybir.dt.int32
AF = mybir.ActivationFunctionType
ALU = mybir.AluOpType
AX = mybir.AxisListType

D = 1024
KC = 8
TT = 512
NB = 4
FD = 2816
NFD = 22
FE = 3584
NFE = 28
NE = 8
CK = 31
HALO = 30
EPS = 1e-6
NEG = -30000.0
NSLOT = 4
NTAP_DVE = 14
SLOT = 4096
ENGS = ["sync", "tensor", "vector", "scalar", "gpsimd"]


class Buf:
    __slots__ = ("name", "w", "r", "rd")

    def __init__(self, name):
        self.name = name
        self.w = None
        self.r = {}
        self.rd = []


class Op:
    __slots__ = ("eng", "fn", "deps", "cnt", "sig", "isdma", "dkey", "dval")

    def __init__(self, eng, fn):
        self.eng = eng
        self.fn = fn
        self.deps = []
        self.cnt = 0
        self.sig = False
        self.isdma = False
        self.dkey = None
        self.dval = 0


class Prog:
    def __init__(self):
        self.q = {e: [] for e in ENGS}
        self.dcnt = {}
        self.nbuf = 0

    def buf(self, name=None):
        self.nbuf += 1
        return Buf(name or ("b%d" % self.nbuf))

    def add(self, eng, fn, reads=(), writes=(), dma_key=None, extra=()):
        op = Op(eng, fn)
        deps = {}

        def dep(d):
            if d is not None and d is not op:
                deps[id(d)] = d

        for b in reads:
            dep(b.w)
        for b in writes:
            w = b.w
            if not (w is not None and w.isdma and dma_key is not None and w.dkey == dma_key):
                dep(w)
            for r in b.r.values():
                dep(r)
            for r in b.rd:
                dep(r)
        for d in extra:
            dep(d)
        op.deps = list(deps.values())
        if fn is None:
            assert not reads and not writes
        if dma_key is not None:
            op.isdma = True
            n = self.dcnt.get(dma_key, 0) + 1
            self.dcnt[dma_key] = n
            op.dkey = dma_key
            op.dval = 16 * n
        for b in reads:
            if op.isdma:
                b.rd.append(op)
            else:
                b.r[eng] = op
        for b in writes:
            b.w = op
            b.r = {}
            b.rd = []
        self.q[eng].append(op)
        return op

    def emit(self, nc):
        for e in ENGS:
            for op in self.q[e]:
                for d in op.deps:
                    if not d.isdma and not (d.eng == "tensor" and e == "tensor"):
                        d.sig = True
        for e in ENGS:
            c = 0
            for op in self.q[e]:
                if op.sig and not op.isdma:
                    c += 1
                    op.cnt = c
        with ExitStack() as st:
            esem = {e: st.enter_context(nc.semaphore("es_" + e)) for e in ENGS}
            dsem = {}
            for i, k in enumerate(self.dcnt):
                dsem[k] = st.enter_context(nc.semaphore("ds%d" % i))
            block = st.enter_context(nc.Block())
            for eng in ENGS:
                ops = self.q[eng]
                if not ops:
                    continue

                def body(e, eng=eng, ops=ops):
                    waited = {}
                    for op in ops:
                        need = {}
                        for d in op.deps:
                            if d.isdma:
                                key = ("d", d.dkey)
                                val = d.dval
                            else:
                                if d.eng == "tensor" and eng == "tensor":
                                    continue
                                key = ("e", d.eng)
                                val = d.cnt
                            if need.get(key, 0) < val:
                                need[key] = val
                        for key, val in need.items():
                            if waited.get(key, 0) >= val:
                                continue
                            sem = dsem[key[1]] if key[0] == "d" else esem[key[1]]
                            e.wait_ge(sem, val)
                            waited[key] = val
                        if op.fn is None:
                            continue
                        ins = op.fn(e)
                        if op.isdma:
                            ins.then_inc(dsem[op.dkey], 16)
                        elif op.sig:
                            ins.then_inc(esem[eng], 1)

                getattr(block, eng)(body)


def make_consts():
    c = np.zeros((128, 1537), np.float32)
    c[:, 0:128] = np.eye(128, dtype=np.float32)
    c[:, 128:256] = 1.0
    bo = np.zeros((128, 128), np.float32)
    bo[:64, :64] = 1.0
    bo[64:, 64:] = 1.0
    c[:, 256:384] = bo
    rp = np.zeros((128, 128), np.float32)
    for hb in (0, 64):
        for m in range(8):
            rp[hb + m + 8, hb + m] = -1.0
            rp[hb + m, hb + m + 8] = 1.0
    c[:, 384:512] = rp
    j = np.arange(128)[:, None]
    q = np.arange(128)[None, :]
    cur = np.where(j <= q, 0.0, NEG).astype(np.float32)
    prev = np.where(j > q, 0.0, NEG).astype(np.float32)
    c[:, 512:1024] = np.tile(cur, (1, 4))
    c[:, 1024:1536] = np.tile(prev, (1, 4))
    inv = (500000.0 ** (-np.arange(0, 16, 2, dtype=np.float32) / 16)).astype(np.float32)
    for p in range(128):
        if p % 64 < 16:
            c[p, 1536] = inv[p % 8]
    return c


def q_chunk_heads(c):
    pair, i = c // 4, c % 4
    return 4 * (2 * pair) + i, 4 * (2 * pair + 1) + i


def build(S, layers, n_cores_dummy=None):
    NT = S // TT
    nc = bass.Bass("TRN2", target_bir_lowering=False, )
    P = Prog()

    def din(name, shape, dt=F32):
        return nc.dram_tensor(name, list(shape), dt, kind="ExternalInput").ap()

    x_d = din("x", [S, D])
    c_d = din("c", [1, D])
    pos_d = din("positions", [1, S], I32)
    cst_d = din("cst", [128, 257])
    cst2_d = din("cst2", [128, 1280])
    norm_g_d = din("norm_g", [4, 2, D])
    w_ada_d = din("w_ada", [4, D, 6 * D])
    b_ada_d = din("b_ada", [4, 6 * D])
    cw_pw1_d = din("conv_w_pw1", [2, D, 2 * D])
    cb_pw1_d = din("conv_b_pw1", [2, 2 * D])
    cw_dw_d = din("conv_w_dw", [2, CK, D])
    cb_dw_d = din("conv_b_dw", [2, D])
    cln_g_d = din("conv_ln_g", [2, D])
    cln_b_d = din("conv_ln_b", [2, D])
    cw_pw2_d = din("conv_w_pw2", [2, D, D])
    cb_pw2_d = din("conv_b_pw2", [2, D])
    a_qkv_d = din("attn_w_qkv", [2, D, 1536])
    a_qg_d = din("attn_q_gain", [2, 64])
    a_kg_d = din("attn_k_gain", [2, 64])
    a_sink_d = din("attn_sinks", [2, 16])
    a_wo_d = din("attn_w_o", [2, D, D])
    f_g_d = din("ffn_w_gate", [2, D, FD])
    f_u_d = din("ffn_w_up", [2, D, FD])
    f_d_d = din("ffn_w_down", [2, FD, D])
    m_r_d = din("moe_w_router", [2, D, NE])
    m_rb_d = din("moe_b_router", [2, NE])
    m_g_d = din("moe_w_gate", [2, NE, D, FE])
    m_u_d = din("moe_w_up", [2, NE, D, FE])
    m_d_d = din("moe_w_down", [2, NE, FE, D])
    y_d = nc.dram_tensor("y", [S, D], F32, kind="ExternalOutput").ap()

    def dscr(name, shape):
        return nc.dram_tensor(name, list(shape), BF16, kind="Internal").ap()

    scr = {}
    for L in layers:
        j = L // 2
        if L % 2 == 0:
            scr[("pw1", j)] = dscr("s_pw1_%d" % j, [4, 128, 8 * 512])
            scr[("pw2", j)] = dscr("s_pw2_%d" % j, [2, 128, 8 * 512])
            scr[("fgu", j)] = dscr("s_fgu_%d" % j, [11, 128, 8 * 512])
            scr[("fd", j)] = dscr("s_fd_%d" % j, [8, 128, NFD * 128])
        else:
            scr[("q", j)] = dscr("s_q_%d" % j, [2, 128, 8 * 512])
            scr[("kv", j)] = dscr("s_kv_%d" % j, [1, 128, 8 * 512])
            scr[("wo", j)] = dscr("s_wo_%d" % j, [2, 128, 8 * 512])
            scr[("mgu", j)] = dscr("s_mgu_%d" % j, [NE, 14, 128, 8 * 512])
            scr[("md", j)] = dscr("s_md_%d" % j, [NE, 8, 128, NFE * 128])

    st = ExitStack()
    with st:
        def T(name, shape, dt=F32):
            return st.enter_context(nc.sbuf_tensor("sb_" + name, list(shape), dt))

        def PS(name, shape):
            return st.enter_context(nc.psum_tensor("ps_" + name, list(shape), F32))

        cst = T("cst", [128, 257])
        cbf = T("cbf", [128, 1536], BF16)
        ident_f = cst[:, 0:128]
        ones_f = cst[:, 128:256]
        invf = cst[:, 256:257]
        ident_b = cbf[:, 0:128]
        ones_b = cbf[:, 128:256]
        bones_b = cbf[:, 256:384]
        rotp_b = cbf[:, 384:512]
        mcur_b = cbf[:, 512:1024]
        mprev_b = cbf[:, 1024:1536]

        NV = 192 + 64 + 32 + 64 + 496 + 8 + 4
        vecT = T("vecT", [128, NV])
        V_BADA, V_NG, V_BPW1, V_BDW, V_LNG, V_LNB, V_BPW2, V_WDW, V_C, V_GAIN = 0, 192, 256, 288, 304, 320, 336, 352, 848, 856
        vrow = [T("vrow0", [128, 128]), T("vrow1", [128, 128])]
        modT = T("modT", [128, 4 * 48])
        coef = T("coef", [128, 4, 3, 8])
        cact2 = T("cact2", [128, 8, 2])
        sinkx = T("sinkx", [128, 2, 16])
        brt = T("brt", [128, 2, 8])
        wr_f = T("wr_f", [128, 2, 8, 8])
        wr_b = T("wr_b", [128, 2, 8, 8], BF16)

        x_sb = T("x_sb", [128, 8, TT])
        tmpA = T("tmpA", [128, 8, TT])
        xio = tmpA[:].rearrange("p c t -> p (c t)").rearrange("p (j d) -> p j d", j=4)
        u32 = T("u32", [128, 8, HALO + TT])
        halo = T("halo", [128, 2, 8, HALO])
        h_bf = T("h_bf", [128, 8, TT], BF16)
        sq_bf = T("sq_bf", [128, 8, TT], BF16)
        q_sb = sq_bf[:].rearrange("p c t -> p (c t)").rearrange("p (a b i q) -> p a b i q", a=2, b=4, i=4)
        o_bf = T("o_bf", [128, 8, TT], BF16)
        act_bf = T("act_bf", [128, NFE, TT], BF16)
        sm = [T("sm%d" % i, [128, TT]) for i in range(6)]
        sil = [T("sil%d" % i, [128, TT]) for i in range(2)]
        tc32 = [T("tc%d" % i, [128, TT]) for i in range(2)]
        qn_bf = T("qn_bf", [128, TT], BF16)
        qn_bf2 = T("qn_bf2", [128, TT], BF16)
        Ctab = T("Ctab", [128, TT])
        Stab = T("Stab", [128, TT])
        pos_i = T("pos_i", [128, TT], I32)
        kz = T("kz", [128, 2, 2, 2, 128 + TT], BF16)
        vaug = T("vaug", [128, 2, 5, 4, 128], BF16)
        pT = [T("pT%d" % i, [128, 2, TT], BF16) for i in range(2)]
        lg = T("lg", [128, 4, 8])
        lg2 = T("lg2", [128, 4, 8])
        eq1 = T("eq1", [128, 4, 8])
        cwt = T("cwt", [128, 4, 8])
        m1 = T("m1", [128, 4])
        m2 = T("m2", [128, 4])
        dg = [T("dg%d" % i, [128, 4, 128]) for i in range(2)]
        wsb = T("wsb", [128, NSLOT * SLOT // 2])
        slots = [wsb[:, i * (SLOT // 2):(i + 1) * (SLOT // 2)].bitcast(BF16) for i in range(NSLOT)]

        psA = PS("psA", [128, 2 * TT])
        psB = PS("psB", [128, 2 * TT])
        psC = [PS("psC0", [128, TT]), PS("psC1", [128, TT])]
        psS = PS("psS", [128, TT])
        psT = PS("psT", [128, TT])

        B = P.buf
        b_cst, b_cbf, b_vecT, b_modT, b_coef, b_cact, b_sink, b_brt, b_wrf, b_wrb = (B() for _ in range(10))
        b_vrow = [B(), B()]
        b_x = [B() for _ in range(8)]
        b_tmpA = [B() for _ in range(8)]
        b_u32 = B()
        b_halo = [B(), B()]
        b_h = [B() for _ in range(8)]
        b_sq = B()
        b_o = [B() for _ in range(8)]
        b_act = [B() for _ in range(NFE)]
        b_sm = [B() for _ in range(6)]
        b_sil = [B(), B()]
        b_tc = [B(), B()]
        b_qn = B()
        b_qn2 = B()
        b_CS = B()
        b_pos = B()
        b_kz = [B(), B()]
        b_va = [B(), B()]
        b_pT = [B(), B()]
        b_lg, b_lg2, b_eq1, b_cwt, b_m1, b_m2 = (B() for _ in range(6))
        b_dg = [B(), B()]
        b_slot = [B() for _ in range(NSLOT)]
        b_A = [B(), B()]
        b_B = [B(), B()]
        b_C = [B(), B()]
        b_S = B()
        b_T = B()
        psAh = [psA[:, 0:TT], psA[:, TT:2 * TT]]
        psBh = [psB[:, 0:TT], psB[:, TT:2 * TT]]

        def MM(out, lhsT, rhs, start, stop, rd, wr):
            return P.add("tensor", lambda e: e.matmul(out, lhsT=lhsT, rhs=rhs, start=start, stop=stop), reads=rd, writes=wr)

        def TR(out, in_, ident, rd, wr):
            return P.add("tensor", lambda e: e.transpose(out=out, in_=in_, identity=ident), reads=rd, writes=wr)

        def ACT(out, in_, func, rd, wr, scale=1.0, bias=None):
            if bias is None:
                return P.add("scalar", lambda e: e.activation(out=out, in_=in_, func=func, scale=scale), reads=rd, writes=wr)
            return P.add("scalar", lambda e: e.activation(out=out, in_=in_, func=func, scale=scale, bias=bias), reads=rd, writes=wr)

        def TS(eng, out, in0, s1, op0, rd, wr, s2=None, op1=None):
            if op1 is None:
                return P.add(eng, lambda e: e.tensor_scalar(out=out, in0=in0, scalar1=s1, scalar2=None, op0=op0), reads=rd, writes=wr)
            return P.add(eng, lambda e: e.tensor_scalar(out=out, in0=in0, scalar1=s1, scalar2=s2, op0=op0, op1=op1), reads=rd, writes=wr)

        def STT(out, in0, scalar, in1, op0, op1, rd, wr):
            return P.add("vector", lambda e: e.scalar_tensor_tensor(out=out, in0=in0, scalar=scalar, in1=in1, op0=op0, op1=op1), reads=rd, writes=wr)

        def TTo(eng, out, in0, in1, op, rd, wr):
            return P.add(eng, lambda e: e.tensor_tensor(out=out, in0=in0, in1=in1, op=op), reads=rd, writes=wr)

        def CP(eng, out, in_, rd, wr):
            if eng == "scalar":
                return P.add(eng, lambda e: e.activation(out=out, in_=in_, func=AF.Copy), reads=rd, writes=wr)
            return P.add(eng, lambda e: e.tensor_copy(out=out, in_=in_), reads=rd, writes=wr)

        def RECIP(out, in_, rd, wr):
            return P.add("vector", lambda e: e.reciprocal(out=out, in_=in_), reads=rd, writes=wr)

        def MSET(eng, ap, val, wr):
            return P.add(eng, lambda e: e.memset(ap, val), writes=wr)

        def DMA(eng, out, in_, rd, wr, key, extra=()):
            return P.add(eng, lambda e: e.dma_start(out=out, in_=in_), reads=rd, writes=wr, dma_key=key, extra=extra)

        DMA("sync", cst[:], cst_d, [], [b_cst], "cst")
        DMA("sync", tmpA[:, 0:3, :].rearrange("p a b -> p (a b)")[:, 0:1280], cst2_d, [], b_tmpA, "cst2")
        CP("vector", cbf[:, 0:256], cst[:, 0:256], [b_cst], [b_cbf])
        CP("vector", cbf[:, 256:1536], tmpA[:, 0:3, :].rearrange("p a b -> p (a b)")[:, 0:1280], b_tmpA, [b_cbf])
        for i in range(2):
            MSET("vector", vrow[i][:], 0.0, [b_vrow[i]])
        MSET("vector", halo[:], 0.0, b_halo)
        MSET("gpsimd", kz[:], 0.0, b_kz)
        MSET("gpsimd", vaug[:], 1.0, b_va)

        vcount = [0]

        def load_vec(src2d, nrows, col0):
            r0 = 0
            while r0 < nrows:
                n = min(128, nrows - r0)
                i = vcount[0] % 2
                vcount[0] += 1
                DMA("sync", vrow[i][0:n, :], src2d[r0:r0 + n, :], [], [b_vrow[i]], ("vrow", i))
                TR(psT[:, 0:128], vrow[i][:], ident_f, [b_vrow[i], b_cst], [b_T])
                CP("vector", vecT[:, col0 + r0:col0 + r0 + n], psT[:, 0:n], [b_T], [b_vecT])
                r0 += n

        load_vec(b_ada_d.rearrange("i (r p) -> (i r) p", p=128), 192, V_BADA)
        load_vec(norm_g_d.rearrange("i s (r p) -> (i s r) p", p=128), 64, V_NG)
        load_vec(cb_pw1_d.rearrange("l (r p) -> (l r) p", p=128), 32, V_BPW1)
        load_vec(cb_dw_d.rearrange("l (r p) -> (l r) p", p=128), 16, V_BDW)
        load_vec(cln_g_d.rearrange("l (r p) -> (l r) p", p=128), 16, V_LNG)
        load_vec(cln_b_d.rearrange("l (r p) -> (l r) p", p=128), 16, V_LNB)
        load_vec(cb_pw2_d.rearrange("l (r p) -> (l r) p", p=128), 16, V_BPW2)
        load_vec(cw_dw_d.rearrange("l k (r p) -> (l k r) p", p=128), 496, V_WDW)
        load_vec(c_d.rearrange("o (r p) -> (o r) p", p=128), 8, V_C)
        i = vcount[0] % 2
        vcount[0] += 1
        for l in range(2):
            for which, gd in enumerate((a_qg_d, a_kg_d)):
                for hf in range(2):
                    DMA("sync", vrow[i][l * 2 + which:l * 2 + which + 1, hf * 64:(hf + 1) * 64], gd[l:l + 1, :], [], [b_vrow[i]], ("vrow", i))
        TR(psT[:, 0:128], vrow[i][:], ident_f, [b_vrow[i], b_cst], [b_T])
        CP("vector", vecT[:, V_GAIN:V_GAIN + 4], psT[:, 0:4], [b_T], [b_vecT])

        for l in range(2):
            DMA("sync", sinkx[:, l, :], a_sink_d[l:l + 1, :].partition_broadcast(128), [], [b_sink], "sink")
            DMA("sync", brt[:, l, :], m_rb_d[l:l + 1, :].partition_broadcast(128), [], [b_brt], "brt")
            DMA("sync", wr_f[:, l, :, :], m_r_d[l].rearrange("(kc p) e -> p kc e", p=128), [], [b_wrf], "wrf")
        ACT(sinkx[:], sinkx[:], AF.Exp, [b_sink], [b_sink])
        CP("vector", wr_b[:], wr_f[:], [b_wrf], [b_wrb])

        ACT(cact2[:, :, 0], vecT[:, V_C:V_C + 8], AF.Silu, [b_vecT], [b_cact])
        CP("vector", cact2[:, :, 1], cact2[:, :, 0], [b_cact], [b_cact])

        stage_w = [(tmpA, b_tmpA), (u32, [b_u32])]
        wk = 0
        for L in layers:
            for blk in range(12):
                tl, tb = stage_w[wk % 2]
                wk += 1
                DMA("sync", tl[:, :, 0:512], w_ada_d[L, :, blk * 512:(blk + 1) * 512].rearrange("(kc p) n -> p kc n", p=128),
                    [], tb, ("wada", wk % 2))
                for cc in range(4):
                    m = blk * 4 + cc
                    for kc in range(KC):
                        MM(psS[:, 2 * m:2 * m + 2], tl[:, kc, cc * 128:(cc + 1) * 128], cact2[:, kc, :], kc == 0, kc == KC - 1,
                           tb + [b_cact], [b_S])
            TTo("vector", modT[:, L * 48:(L + 1) * 48], psS[:, 0:96].rearrange("p (m two) -> p m two", two=2)[:, :, 0],
                vecT[:, V_BADA + L * 48:V_BADA + (L + 1) * 48], ALU.add, [b_S, b_vecT], [b_modT])
            for s in range(2):
                STT(coef[:, L, s, :], modT[:, L * 48 + (3 * s + 1) * 8:L * 48 + (3 * s + 2) * 8], 1.0,
                    vecT[:, V_NG + L * 16 + s * 8:V_NG + L * 16 + (s + 1) * 8], ALU.add, ALU.mult, [b_modT, b_vecT], [b_coef])
            if L % 2 == 0:
                j = L // 2
                TTo("vector", coef[:, L, 2, :], modT[:, L * 48 + 16:L * 48 + 24], vecT[:, V_BPW2 + j * 8:V_BPW2 + (j + 1) * 8],
                    ALU.mult, [b_modT, b_vecT], [b_coef])

        def mod(L, jj, cc):
            return modT[:, L * 48 + jj * 8 + cc:L * 48 + jj * 8 + cc + 1]

        cast_engs = ["vector", "scalar", "gpsimd"]
        pq = []
        cvt = {"k": 0, "stores": []}
        fst = [wsb[:, i * SLOT:i * SLOT + FE] for i in range(2)]
        b_fst = [B() for _ in range(2)]
        bstv = [act_bf[:, 7 * i:7 * i + 7, :].rearrange("p a b -> p (a b)") for i in range(4)]
        b_bst = [b_act[7 * i:7 * i + 7] for i in range(4)]

        def convert(src_rows, ncols, store_fn):
            k = cvt["k"]
            cvt["k"] += 1
            f, bf = fst[k % 2], b_fst[k % 2]
            g, bg = bstv[k % 4], b_bst[k % 4]
            DMA("sync", f[:, 0:ncols], src_rows, [], [bf], ("fst", k % 2))
            eng = cast_engs[k % 3]
            CP(eng, g[:, 0:ncols], f[:, 0:ncols], [bf], bg)
            pq.append((k, g, bg, store_fn))
            if len(pq) > 2:
                flush_one()

        def flush_one():
            k, g, bg, store_fn = pq.pop(0)
            for (dst, src) in store_fn(g):
                cvt["stores"].append(DMA("sync", dst, src, bg, [], ("bst", k % 4)))

        def a_store(sc, rb, W, entries):
            def fn(g):
                out = []
                for (g0, ng, dcol0, scol0, ncols, sstride) in entries:
                    if ng == 1:
                        out.append((sc[g0, :, rb * W + dcol0:rb * W + dcol0 + ncols], g[:, scol0:scol0 + ncols]))
                    else:
                        dst = sc[g0:g0 + ng, :, rb * W + dcol0:rb * W + dcol0 + ncols].rearrange("g p w -> p g w")
                        src = g[:, scol0:scol0 + ng * sstride].rearrange("p (g s) -> p g s", s=sstride)[:, :, 0:ncols]
                        out.append((dst, src))
                return out
            return fn

        for L in layers:
            j = L // 2
            if L % 2 == 0:
                for rb in range(8):
                    rows = slice(rb * 128, (rb + 1) * 128)
                    convert(cw_pw1_d[j, rows, :], 2048, a_store(scr[("pw1", j)], rb, 512,
                            [(0, 4, 0, 0, 256, 256), (0, 4, 256, 1024, 256, 256)]))
                    convert(cw_pw2_d[j, rows, :], 1024, a_store(scr[("pw2", j)], rb, 512, [(0, 2, 0, 0, 512, 512)]))
                    convert(f_g_d[j, rows, :], FD, a_store(scr[("fgu", j)], rb, 512, [(0, 11, 0, 0, 256, 256)]))
                    convert(f_u_d[j, rows, :], FD, a_store(scr[("fgu", j)], rb, 512, [(0, 11, 256, 0, 256, 256)]))
                for rb in range(NFD):
                    rows = slice(rb * 128, (rb + 1) * 128)
                    convert(f_d_d[j, rows, :], 1024, a_store(scr[("fd", j)], rb, 128, [(0, 8, 0, 0, 128, 128)]))
            else:
                for rb in range(8):
                    rows = slice(rb * 128, (rb + 1) * 128)
                    ents = []
                    for c in range(8):
                        ha, hb_ = q_chunk_heads(c)
                        ents.append((c // 4, 1, (c % 4) * 128, ha * 64, 64, 0))
                        ents.append((c // 4, 1, (c % 4) * 128 + 64, hb_ * 64, 64, 0))
                    ents.append((0, 1, 0, 1024, 512, 0))
                    sq_, skv = scr[("q", j)], scr[("kv", j)]

                    def qkv_store(g, rb=rb, ents=ents, sq_=sq_, skv=skv):
                        out = []
                        for (g0, ng, dcol0, scol0, ncols, _s) in ents[:-1]:
                            out.append((sq_[g0, :, rb * 512 + dcol0:rb * 512 + dcol0 + ncols], g[:, scol0:scol0 + ncols]))
                        out.append((skv[0, :, rb * 512:rb * 512 + 512], g[:, 1024:1536]))
                        return out
                    convert(a_qkv_d[j, rows, :], 1536, qkv_store)
                    convert(a_wo_d[j, rows, :], 1024, a_store(scr[("wo", j)], rb, 512, [(0, 2, 0, 0, 512, 512)]))
        while pq:
            flush_one()
        P.add("sync", None, extra=cvt["stores"])
        ws = {"k": 0}

        def wload(src, n):
            i = ws["k"] % NSLOT
            ws["k"] += 1
            DMA("sync", slots[i][:, 0:n], src, [], [b_slot[i]], ("ws", i))
            return slots[i], b_slot[i]

        hs = [tmpA[:, 0:4, :].rearrange("p a b -> p (a b)"), tmpA[:, 4:8, :].rearrange("p a b -> p (a b)"),
              sq_bf[:].rearrange("p a b -> p (a b)").bitcast(F32), o_bf[:].rearrange("p a b -> p (a b)").bitcast(F32)]
        b_hs = [b_tmpA[0:4], b_tmpA[4:8], [b_sq], b_o]
        hs_rr = [0]
        cast_rr = [0]
        inl_stores = []
        pend_st = []

        def flush_st():
            dst, slot, n, bs, i = pend_st.pop(0)
            inl_stores.append(DMA("sync", dst, slot[:, 0:n], [bs], [], ("wst", i)))

        def issue_conv(kind, j, ex, idx):
            i = ws["k"] % NSLOT
            ws["k"] += 1
            slot, bs = slots[i], b_slot[i]
            if kind == "gu":
                sv = slot[:].rearrange("p (kc n) -> p kc n", kc=8)
                for (wd, col0) in ((m_g_d, 0), (m_u_d, 256)):
                    h = hs_rr[0] % 4
                    hs_rr[0] += 1
                    hv = hs[h].rearrange("p (kc n) -> p kc n", kc=8)
                    DMA("sync", hv, wd[j, ex, :, idx * 256:(idx + 1) * 256].rearrange("(kc p) n -> p kc n", p=128), [], b_hs[h], ("hs", h))
                    eng = cast_engs[cast_rr[0] % 3]
                    cast_rr[0] += 1
                    CP(eng, sv[:, :, col0:col0 + 256], hv, b_hs[h], [bs])
                n = 4096
                dst = scr[("mgu", j)][ex][idx]
            else:
                src = m_d_d[j, ex, :, idx * 128:(idx + 1) * 128].rearrange("(f p) n -> p f n", p=128)
                for (f0, f1) in ((0, 16), (16, NFE)):
                    h = hs_rr[0] % 4
                    hs_rr[0] += 1
                    hv = hs[h][:, 0:(f1 - f0) * 128].rearrange("p (f n) -> p f n", n=128)
                    DMA("sync", hv, src[:, f0:f1, :], [], b_hs[h], ("hs", h))
                    eng = cast_engs[cast_rr[0] % 3]
                    cast_rr[0] += 1
                    CP(eng, slot[:, f0 * 128:f1 * 128].rearrange("p (f n) -> p f n", n=128), hv, b_hs[h], [bs])
                n = NFE * 128
                dst = scr[("md", j)][ex][idx]
            pend_st.append((dst, slot, n, bs, i))
            if len(pend_st) > 2:
                flush_st()
            return slot, bs

        act_flat = act_bf[:].rearrange("p a b -> p (a b)")
        u_bf = act_flat[:, 0:8 * 544].rearrange("p (c t) -> p c t", c=8)
        o_flat = o_bf[:].rearrange("p a b -> p (a b)")
        sq_flat = sq_bf[:].rearrange("p a b -> p (a b)")

        def diag_loc(m):
            if m < 76:
                off = 9 * 512 + m * 128
                f0 = off // 512
                f1 = (off + 127) // 512
                return act_flat[:, off:off + 128], b_act[f0:f1 + 1]
            if m < 108:
                off = (m - 76) * 128
                return o_flat[:, off:off + 128], [b_o[off // 512]]
            off = (m - 108) * 128
            return sq_flat[:, off:off + 128], [b_sq]

        def rmsnorm_mod(L, s):
            ACT(sq_bf[:], x_sb[:], AF.Square, b_x, [b_sq])
            for c in range(KC):
                MM(psS[:], ones_b, sq_bf[:, c, :], c == 0, c == KC - 1, [b_sq, b_cbf], [b_S])
            ACT(sm[0][:], psS[:], AF.Ln, [b_S], [b_sm[0]], scale=1.0 / D, bias=EPS)
            ACT(sm[0][:], sm[0][:], AF.Exp, [b_sm[0]], [b_sm[0]], scale=-0.5)
            for c in range(KC):
                STT(tmpA[:, c, :], x_sb[:, c, :], coef[:, L, s, c:c + 1], sm[0][:], ALU.mult, ALU.mult,
                    [b_x[c], b_coef, b_sm[0]], [b_tmpA[c]])
            for c in range(KC):
                ACT(h_bf[:, c, :], tmpA[:, c, :], AF.Identity, [b_tmpA[c], b_modT], [b_h[c]], bias=mod(L, 3 * s, c))

        def evac_residual(ps, bps, L, jj, c, eng_add="gpsimd", extra_scale=None, ti=0):
            if extra_scale is None:
                STT(x_sb[:, c, :], ps, mod(L, jj, c), x_sb[:, c, :], ALU.mult, ALU.add, [bps, b_modT, b_x[c]], [b_x[c]])
            else:
                es, bes = extra_scale
                STT(tc32[ti][:], ps, mod(L, jj, c), es, ALU.mult, ALU.mult, [bps, b_modT, bes], [b_tc[ti]])
                TTo("gpsimd", x_sb[:, c, :], x_sb[:, c, :], tc32[ti][:], ALU.add, [b_x[c], b_tc[ti]], [b_x[c]])

        def conv_layer(L, t):
            j = L // 2
            rmsnorm_mod(L, 0)
            for ti_, kk in enumerate(range(NTAP_DVE, CK)):
                for c in range(KC):
                    dgm, bdg = diag_loc(ti_ * 8 + c)
                    TS("vector", dgm, ident_b, vecT[:, V_WDW + j * 248 + kk * 8 + c:V_WDW + j * 248 + kk * 8 + c + 1], ALU.mult,
                       [b_cbf, b_vecT], bdg)
            CP("gpsimd", u32[:, :, 0:HALO], halo[:, j, :, :], [b_halo[j]], [b_u32])
            k = 0
            for g in range(4):
                w, bw = wload(scr[("pw1", j)][g], 4096)
                wv = w[:].rearrange("p (kc n) -> p kc n", kc=8)
                for cc in range(2):
                    c = 2 * g + cc
                    pa, bpa = psAh[k % 2], b_A[k % 2]
                    pg, bpg = psBh[k % 2], b_B[k % 2]
                    for kc in range(KC):
                        MM(pa, wv[:, kc, cc * 128:(cc + 1) * 128], h_bf[:, kc, :], kc == 0, kc == KC - 1, [bw, b_h[kc]], [bpa])
                    for kc in range(KC):
                        MM(pg, wv[:, kc, 256 + cc * 128:256 + (cc + 1) * 128], h_bf[:, kc, :], kc == 0, kc == KC - 1, [bw, b_h[kc]], [bpg])
                    ACT(sil[k % 2][:], pg, AF.Sigmoid, [bpg, b_vecT], [b_sil[k % 2]],
                        bias=vecT[:, V_BPW1 + j * 16 + 8 + c:V_BPW1 + j * 16 + 8 + c + 1])
                    STT(u32[:, c, HALO:HALO + TT], pa, vecT[:, V_BPW1 + j * 16 + c:V_BPW1 + j * 16 + c + 1], sil[k % 2][:],
                        ALU.add, ALU.mult, [bpa, b_vecT, b_sil[k % 2]], [b_u32])
                    k += 1
            for c in range(KC):
                CP("scalar", u_bf[:, c, 0:HALO + TT], u32[:, c, :], [b_u32], b_act[0:9])
            for kk in range(NTAP_DVE):
                for c in range(KC):
                    wcol = vecT[:, V_WDW + j * 248 + kk * 8 + c:V_WDW + j * 248 + kk * 8 + c + 1]
                    if kk == 0:
                        TS("vector", tmpA[:, c, :], u32[:, c, 0:TT], wcol, ALU.mult, [b_u32, b_vecT], [b_tmpA[c]],
                           s2=vecT[:, V_BDW + j * 8 + c:V_BDW + j * 8 + c + 1], op1=ALU.add)
                    else:
                        STT(tmpA[:, c, :], u32[:, c, kk:kk + TT], wcol, tmpA[:, c, :], ALU.mult, ALU.add,
                            [b_u32, b_vecT, b_tmpA[c]], [b_tmpA[c]])
            for c in range(KC):
                pc, bpc = psC[c % 2], b_C[c % 2]
                for ti_, kk in enumerate(range(NTAP_DVE, CK)):
                    dgm, bdg = diag_loc(ti_ * 8 + c)
                    MM(pc[:], dgm, u_bf[:, c, kk:kk + TT], ti_ == 0, kk == CK - 1, bdg + b_act[0:9], [bpc])
                TTo("vector", tmpA[:, c, :], pc[:], tmpA[:, c, :], ALU.add, [bpc, b_tmpA[c]], [b_tmpA[c]])
            CP("gpsimd", halo[:, j, :, :], u32[:, :, TT:TT + HALO], [b_u32], [b_halo[j]])
            CP("scalar", h_bf[:], tmpA[:], b_tmpA, b_h)
            ACT(sq_bf[:], tmpA[:], AF.Square, b_tmpA, [b_sq])
            for c in range(KC):
                MM(psS[:], ones_b, h_bf[:, c, :], c == 0, c == KC - 1, [b_h[c], b_cbf], [b_S])
            for c in range(KC):
                MM(psT[:], ones_b, sq_bf[:, c, :], c == 0, c == KC - 1, [b_sq, b_cbf], [b_T])
            TS("vector", sm[1][:], psS[:], 1.0 / D, ALU.mult, [b_S], [b_sm[1]])
            TTo("vector", sm[2][:], sm[1][:], sm[1][:], ALU.mult, [b_sm[1]], [b_sm[2]])
            STT(sm[2][:], psT[:], 1.0 / D, sm[2][:], ALU.mult, ALU.subtract, [b_T, b_sm[2]], [b_sm[2]])
            TS("vector", sm[2][:], sm[2][:], 0.0, ALU.max, [b_sm[2]], [b_sm[2]])
            ACT(sm[2][:], sm[2][:], AF.Ln, [b_sm[2]], [b_sm[2]], bias=EPS)
            ACT(sm[2][:], sm[2][:], AF.Exp, [b_sm[2]], [b_sm[2]], scale=-0.5)
            for c in range(KC):
                TTo("vector", tmpA[:, c, :], tmpA[:, c, :], sm[1][:], ALU.subtract, [b_tmpA[c], b_sm[1]], [b_tmpA[c]])
                TTo("vector", tmpA[:, c, :], tmpA[:, c, :], sm[2][:], ALU.mult, [b_tmpA[c], b_sm[2]], [b_tmpA[c]])
            for c in range(KC):
                P.add("scalar", lambda e, c=c: e.activation(
                    out=o_bf[:, c, :], in_=tmpA[:, c, :], func=AF.Silu,
                    scale=vecT[:, V_LNG + j * 8 + c:V_LNG + j * 8 + c + 1],
                    bias=vecT[:, V_LNB + j * 8 + c:V_LNB + j * 8 + c + 1]), reads=[b_tmpA[c], b_vecT], writes=[b_o[c]])
            k = 0
            for g in range(2):
                w, bw = wload(scr[("pw2", j)][g], 4096)
                wv = w[:].rearrange("p (kc n) -> p kc n", kc=8)
                for cc in range(4):
                    c = 4 * g + cc
                    pc, bpc = psC[k % 2], b_C[k % 2]
                    for kc in range(KC):
                        MM(pc[:], wv[:, kc, cc * 128:(cc + 1) * 128], o_bf[:, kc, :], kc == 0, kc == KC - 1, [bw, b_o[kc]], [bpc])
                    evac_residual(pc[:], bpc, L, 2, c)
                    TS("gpsimd", x_sb[:, c, :], x_sb[:, c, :], coef[:, L, 2, c:c + 1], ALU.add, [b_x[c], b_coef], [b_x[c]])
                    k += 1

        def swiglu(L, j, gu_scr, d_scr, nf, jj, extra_scale=None):
            k = 0
            for g in range(nf // 2):
                w, bw = wload(gu_scr[g], 4096)
                wv = w[:].rearrange("p (kc n) -> p kc n", kc=8)
                for cc in range(2):
                    f = 2 * g + cc
                    pg, bpg = psAh[k % 2], b_A[k % 2]
                    pu, bpu = psBh[k % 2], b_B[k % 2]
                    for kc in range(KC):
                        MM(pg, wv[:, kc, cc * 128:(cc + 1) * 128], h_bf[:, kc, :], kc == 0, kc == KC - 1, [bw, b_h[kc]], [bpg])
                    for kc in range(KC):
                        MM(pu, wv[:, kc, 256 + cc * 128:256 + (cc + 1) * 128], h_bf[:, kc, :], kc == 0, kc == KC - 1, [bw, b_h[kc]], [bpu])
                    ACT(sil[k % 2][:], pg, AF.Silu, [bpg], [b_sil[k % 2]])
                    TTo("vector", act_bf[:, f, :], pu, sil[k % 2][:], ALU.mult, [bpu, b_sil[k % 2]], [b_act[f]])
                    k += 1
            for c in range(KC):
                w, bw = wload(d_scr[c], nf * 128)
                wv = w[:, 0:nf * 128].rearrange("p (f n) -> p f n", n=128)
                pc, bpc = psC[c % 2], b_C[c % 2]
                for f in range(nf):
                    MM(pc[:], wv[:, f, :], act_bf[:, f, :], f == 0, f == nf - 1, [bw, b_act[f]], [bpc])
                evac_residual(pc[:], bpc, L, jj, c, extra_scale=extra_scale, ti=c % 2)

        def rope_tables(t):
            t0 = t * TT
            DMA("sync", pos_i[:], pos_d[:, t0:t0 + TT].partition_broadcast(128), [], [b_pos], "pos")
            CP("vector", sm[3][:], pos_i[:], [b_pos], [b_sm[3]])
            TS("vector", sm[3][:], sm[3][:], invf, ALU.mult, [b_sm[3], b_cst], [b_sm[3]])
            M = 12582912.0
            HI = 6.28125
            LO = 2.0 * np.pi - 6.28125
            for which, tab in ((0, Stab), (1, Ctab)):
                src = sm[3]
                if which == 1:
                    TS("vector", sm[4][:], sm[3][:], float(np.pi / 2), ALU.add, [b_sm[3]], [b_sm[4]])
                    src = sm[4]
                bsrc = b_sm[3] if which == 0 else b_sm[4]
                TS("vector", sm[5][:], src[:], float(1.0 / (2 * np.pi)), ALU.mult, [bsrc], [b_sm[5]])
                TS("vector", sm[5][:], sm[5][:], M, ALU.add, [b_sm[5]], [b_sm[5]], s2=M, op1=ALU.subtract)
                STT(tab[:], sm[5][:], -HI, src[:], ALU.mult, ALU.add, [b_sm[5], bsrc], [b_CS])
                STT(tab[:], sm[5][:], -float(LO), tab[:], ALU.mult, ALU.add, [b_sm[5], b_CS], [b_CS])
                TS("vector", tab[:], tab[:], 3.141592, ALU.min, [b_CS], [b_CS], s2=-3.141592, op1=ALU.max)
                ACT(tab[:], tab[:], AF.Sin, [b_CS], [b_CS])

        def qk_post(ps, bps, gcol, out_fn, par):
            if par == 0:
                s_r, b_r, s_q, b_q, s_s, b_s, qb, bqb, pst, bpst = sm[1], b_sm[1], sm[2], b_sm[2], sm[4], b_sm[4], qn_bf, b_qn, psT, b_T
            else:
                s_r, b_r, s_q, b_q, s_s, b_s, qb, bqb, pst, bpst = sm[0], b_sm[0], sm[3], b_sm[3], sm[5], b_sm[5], qn_bf2, b_qn2, psS, b_S
            ACT(qb[:], ps, AF.Square, [bps], [bqb])
            MM(pst[:], bones_b, qb[:], True, True, [bqb, b_cbf], [bpst])
            ACT(s_r[:], pst[:], AF.Ln, [bpst], [b_r], scale=1.0 / 64, bias=EPS)
            ACT(s_r[:], s_r[:], AF.Exp, [b_r], [b_r], scale=-0.5)
            STT(s_q[:], ps, vecT[:, gcol:gcol + 1], s_r[:], ALU.mult, ALU.mult, [bps, b_vecT, b_r], [b_q])
            CP("scalar", qb[:], s_q[:], [b_q], [bqb])
            MM(pst[:], rotp_b, qb[:], True, True, [bqb, b_cbf], [bpst])
            TTo("vector", s_q[:], s_q[:], Ctab[:], ALU.mult, [b_q, b_CS], [b_q])
            TTo("vector", s_s[:], pst[:], Stab[:], ALU.mult, [bpst, b_CS], [b_s])
            out_fn(s_q, b_q, s_s, b_s)

        def attn_layer(L, t):
            j = L // 2
            rmsnorm_mod(L, 0)
            k = 0
            for g in range(2):
                w, bw = wload(scr[("q", j)][g], 4096)
                wv = w[:].rearrange("p (kc n) -> p kc n", kc=8)
                for cc in range(4):
                    c = 4 * g + cc
                    pair, ii = c // 4, c % 4
                    pq_, bpq = psC[k % 2], b_C[k % 2]
                    for kc in range(KC):
                        MM(pq_[:], wv[:, kc, cc * 128:(cc + 1) * 128], h_bf[:, kc, :], kc == 0, kc == KC - 1, [bw, b_h[kc]], [bpq])

                    def outq(sq_, bq_, ss_, bs_, pair=pair, ii=ii):
                        TTo("vector", q_sb[:, pair, :, ii, :], sq_[:].rearrange("p (b q) -> p b q", b=4),
                            ss_[:].rearrange("p (b q) -> p b q", b=4), ALU.add, [bq_, bs_], [b_sq])
                    qk_post(pq_[:], bpq, V_GAIN + j * 2 + 0, outq, k % 2)
                    k += 1
            w, bw = wload(scr[("kv", j)][0], 4096)
            wv = w[:].rearrange("p (kc n) -> p kc n", kc=8)
            for pair in range(2):
                pk, bpk = psC[pair % 2], b_C[pair % 2]
                for kc in range(KC):
                    MM(pk[:], wv[:, kc, pair * 128:(pair + 1) * 128], h_bf[:, kc, :], kc == 0, kc == KC - 1, [bw, b_h[kc]], [bpk])

                def outk(sq_, bq_, ss_, bs_, pair=pair):
                    for hh in range(2):
                        TTo("vector", kz[hh * 64:(hh + 1) * 64, j, pair, hh, 128:128 + TT], sq_[hh * 64:(hh + 1) * 64, :],
                            ss_[hh * 64:(hh + 1) * 64, :], ALU.add, [bq_, bs_], [b_kz[j]])
                qk_post(pk[:], bpk, V_GAIN + j * 2 + 1, outk, pair % 2)
            for blk in range(NB):
                pv, bpv = psAh[blk % 2], b_A[blk % 2]
                for kc in range(KC):
                    MM(pv[:, 0:256], h_bf[:, kc, blk * 128:(blk + 1) * 128], wv[:, kc, 256:512], kc == 0, kc == KC - 1, [bw, b_h[kc]], [bpv])
                CP("scalar", vaug[:, j, 1 + blk, :, 0:64], pv[:, 0:256].rearrange("p (g d) -> p g d", g=4), [bpv], [b_va[j]])
            k = 0
            for blk in range(NB):
                first = (t == 0 and blk == 0)
                for pair in range(2):
                    for hh in range(2):
                        g = 2 * pair + hh
                        pss, bps2 = (psA, b_A) if k % 2 == 0 else (psB, b_B)
                        ptile, bpt = pT[k % 2], b_pT[k % 2]
                        rhs_q = q_sb[:, pair, blk, :, :].rearrange("p i q -> p (i q)")
                        kbs = [1] if first else [0, 1]
                        for kb in kbs:
                            col0 = blk * 128 + kb * 128
                            MM(pss[:, kb * TT:(kb + 1) * TT], kz[:, j, pair, hh, col0:col0 + 128], rhs_q, True, False,
                               [b_kz[j], b_sq], [bps2[kb]])
                            MM(pss[:, kb * TT:(kb + 1) * TT], ident_b, (mprev_b if kb == 0 else mcur_b), False, True,
                               [b_cbf], [bps2[kb]])
                        if first:
                            ACT(ptile[:, 1, :], pss[:, TT:2 * TT], AF.Exp, [bps2[1]], [bpt], scale=0.125)
                        else:
                            ACT(ptile[:].rearrange("p a b -> p (a b)"), pss[:], AF.Exp, [bps2[0], bps2[1]], [bpt], scale=0.125)
                        po, bpo = psC[k % 2], b_C[k % 2]
                        for n_, kb in enumerate(kbs):
                            MM(po[:], vaug[:, j, blk + kb, g, :], ptile[:, kb, :], n_ == 0, n_ == len(kbs) - 1, [b_va[j], bpt], [bpo])
                        den, b_den = (sm[1], b_sm[1]) if k % 2 == 0 else (sm[3], b_sm[3])
                        for ii in range(4):
                            h = 4 * g + ii
                            TS("vector", den[64:128, ii * 128:(ii + 1) * 128], po[64:128, ii * 128:(ii + 1) * 128],
                               sinkx[64:128, j, h:h + 1], ALU.add, [bpo, b_sink], [b_den])
                        ACT(den[64:128, :], den[64:128, :], AF.Ln, [b_den], [b_den])
                        ACT(den[64:128, :], den[64:128, :], AF.Exp, [b_den], [b_den], scale=-1.0)
                        for ii in range(4):
                            h = 4 * g + ii
                            ch, hf = h // 2, h % 2
                            TTo("vector", o_bf[hf * 64:(hf + 1) * 64, ch, blk * 128:(blk + 1) * 128],
                                po[0:64, ii * 128:(ii + 1) * 128], den[64:128, ii * 128:(ii + 1) * 128], ALU.mult,
                                [bpo, b_den], [b_o[ch]])
                        k += 1
            CP("gpsimd", kz[:, j, :, :, 0:128], kz[:, j, :, :, TT:TT + 128], [b_kz[j]], [b_kz[j]])
            CP("gpsimd", vaug[:, j, 0, :, 0:64], vaug[:, j, 4, :, 0:64], [b_va[j]], [b_va[j]])
            k = 0
            for g in range(2):
                w, bw = wload(scr[("wo", j)][g], 4096)
                wv = w[:].rearrange("p (kc n) -> p kc n", kc=8)
                for cc in range(4):
                    c = 4 * g + cc
                    pc, bpc = psC[k % 2], b_C[k % 2]
                    for kc in range(KC):
                        MM(pc[:], wv[:, kc, cc * 128:(cc + 1) * 128], o_bf[:, kc, :], kc == 0, kc == KC - 1, [bw, b_o[kc]], [bpc])
                    evac_residual(pc[:], bpc, L, 2, c)
                    k += 1

        def moe_layer(L, t):
            j = L // 2
            rmsnorm_mod(L, 1)
            for blk in range(NB):
                for kc in range(KC):
                    MM(psT[:, blk * 8:(blk + 1) * 8], h_bf[:, kc, blk * 128:(blk + 1) * 128], wr_b[:, j, kc, :], kc == 0, kc == KC - 1,
                       [b_h[kc], b_wrb], [b_T])
            for blk in range(NB):
                TTo("vector", lg[:, blk, :], psT[:, blk * 8:(blk + 1) * 8], brt[:, j, :], ALU.add, [b_T, b_brt], [b_lg])
            P.add("vector", lambda e: e.tensor_reduce(out=m1[:], in_=lg[:], axis=AX.X, op=ALU.max), reads=[b_lg], writes=[b_m1])
            for blk in range(NB):
                TS("vector", eq1[:, blk, :], lg[:, blk, :], m1[:, blk:blk + 1], ALU.is_equal, [b_lg, b_m1], [b_eq1])
            STT(lg2[:].rearrange("p a b -> p (a b)"), eq1[:].rearrange("p a b -> p (a b)"), -1e30, lg[:].rearrange("p a b -> p (a b)"),
                ALU.mult, ALU.add, [b_eq1, b_lg], [b_lg2])
            P.add("vector", lambda e: e.tensor_reduce(out=m2[:], in_=lg2[:], axis=AX.X, op=ALU.max), reads=[b_lg2], writes=[b_m2])
            for blk in range(NB):
                TS("vector", eq1[:, blk, :], lg[:, blk, :], m2[:, blk:blk + 1], ALU.is_ge, [b_lg, b_m2, b_eq1], [b_eq1])
            TS("vector", m1[:], m1[:], -1.0, ALU.mult, [b_m1], [b_m1])
            for blk in range(NB):
                ACT(lg2[:, blk, :], lg[:, blk, :], AF.Exp, [b_lg, b_m1, b_lg2], [b_lg2], bias=m1[:, blk:blk + 1])
            TTo("vector", lg2[:], lg2[:], eq1[:], ALU.mult, [b_lg2, b_eq1], [b_lg2])
            P.add("vector", lambda e: e.tensor_reduce(out=m2[:], in_=lg2[:], axis=AX.X, op=ALU.add), reads=[b_lg2], writes=[b_m2])
            RECIP(m2[:], m2[:], [b_m2], [b_m2])
            for blk in range(NB):
                TS("vector", cwt[:, blk, :], lg2[:, blk, :], m2[:, blk:blk + 1], ALU.mult, [b_lg2, b_m2], [b_cwt])
            for ex in range(NE):
                d_, bd = dg[ex % 2], b_dg[ex % 2]
                for blk in range(NB):
                    TS("vector", d_[:, blk, :], ident_f, cwt[:, blk, ex:ex + 1], ALU.mult, [b_cst, b_cwt], [bd])
                for blk in range(NB):
                    MM(psS[:, blk * 128:(blk + 1) * 128], ones_f, d_[:, blk, :], True, True, [b_cst, bd], [b_S])
                CP("scalar", u32[:, ex, 0:TT], psS[:], [b_S], [b_u32])
            reqs = []
            for ex in range(NE):
                for g in range(NFE // 2):
                    reqs.append(("gu", ex, g))
                for c in range(KC):
                    reqs.append(("d", ex, c))

            def issue(r):
                kind, ex, idx = r
                if t == 0:
                    return issue_conv(kind, j, ex, idx)
                if kind == "gu":
                    return wload(scr[("mgu", j)][ex][idx], 4096)
                return wload(scr[("md", j)][ex][idx], NFE * 128)

            kcnt = 0
            LA = 2
            inflight = [issue(reqs[i]) for i in range(min(LA, len(reqs)))]
            for ri, r in enumerate(reqs):
                w, bw = inflight.pop(0)
                if ri + LA < len(reqs):
                    inflight.append(issue(reqs[ri + LA]))
                kind, ex, idx = r
                if kind == "gu":
                    wv = w[:].rearrange("p (kc n) -> p kc n", kc=8)
                    for cc in range(2):
                        f = 2 * idx + cc
                        pg, bpg = psAh[kcnt % 2], b_A[kcnt % 2]
                        pu, bpu = psBh[kcnt % 2], b_B[kcnt % 2]
                        for kc in range(KC):
                            MM(pg, wv[:, kc, cc * 128:(cc + 1) * 128], h_bf[:, kc, :], kc == 0, kc == KC - 1, [bw, b_h[kc]], [bpg])
                        for kc in range(KC):
                            MM(pu, wv[:, kc, 256 + cc * 128:256 + (cc + 1) * 128], h_bf[:, kc, :], kc == 0, kc == KC - 1, [bw, b_h[kc]], [bpu])
                        ACT(sil[kcnt % 2][:], pg, AF.Silu, [bpg], [b_sil[kcnt % 2]])
                        TTo("vector", act_bf[:, f, :], pu, sil[kcnt % 2][:], ALU.mult, [bpu, b_sil[kcnt % 2]], [b_act[f]])
                        kcnt += 1
                else:
                    c = idx
                    wv = w[:, 0:NFE * 128].rearrange("p (f n) -> p f n", n=128)
                    pc, bpc = psC[c % 2], b_C[c % 2]
                    for f in range(NFE):
                        MM(pc[:], wv[:, f, :], act_bf[:, f, :], f == 0, f == NFE - 1, [bw, b_act[f]], [bpc])
                    evac_residual(pc[:], bpc, L, 5, c, extra_scale=(u32[:, ex, 0:TT], b_u32), ti=c % 2)
            if t == 0:
                while pend_st:
                    flush_st()
                P.add("sync", None, extra=list(inl_stores))

        need_rope = any(L % 2 == 1 for L in layers)
        for t in range(NT):
            t0 = t * TT
            DMA("scalar", xio, x_d[t0:t0 + TT, :].rearrange("(j p) d -> p j d", p=128), [], b_tmpA, "xin")
            for c in range(KC):
                for jb in range(4):
                    TR(psT[:, jb * 128:(jb + 1) * 128], xio[:, jb, c * 128:(c + 1) * 128], ident_f, b_tmpA + [b_cst], [b_T])
                CP("vector" if c % 2 == 0 else "scalar", x_sb[:, c, :], psT[:], [b_T], [b_x[c]])
            if need_rope:
                rope_tables(t)
            for L in layers:
                if L % 2 == 0:
                    conv_layer(L, t)
                    rmsnorm_mod(L, 1)
                    swiglu(L, L // 2, scr[("fgu", L // 2)], scr[("fd", L // 2)], NFD, 5)
                else:
                    attn_layer(L, t)
                    moe_layer(L, t)
            for jb in range(4):
                for c in range(KC):
                    TR(psT[:, (c % 4) * 128:(c % 4 + 1) * 128], x_sb[:, c, jb * 128:(jb + 1) * 128], ident_f, [b_x[c], b_cst], [b_T])
                    if c % 4 == 3:
                        cb = c // 4
                        CP("vector" if cb == 0 else "scalar", xio[:, jb, cb * 512:(cb + 1) * 512], psT[:], [b_T], b_tmpA)
            last_store = DMA("scalar", y_d[t0:t0 + TT, :].rearrange("(j p) d -> p j d", p=128), xio, b_tmpA, [], "xout")
        P.add("scalar", None, extra=[last_store])
        P.emit(nc)
    return nc


_W_NAMES = ["norm_g", "w_ada", "b_ada", "conv_w_pw1", "conv_b_pw1", "conv_w_dw", "conv_b_dw", "conv_ln_g", "conv_ln_b",
            "conv_w_pw2", "conv_b_pw2", "attn_w_qkv", "attn_q_gain", "attn_k_gain", "attn_sinks", "attn_w_o",
            "ffn_w_gate", "ffn_w_up", "ffn_w_down", "moe_w_router", "moe_b_router", "moe_w_gate", "moe_w_up", "moe_w_down"]
_CACHE = {}


def run_layers(x, c, positions, weights, layers, runner=None):
    Bn, S, _ = x.shape
    key = (S, tuple(layers))
    if key not in _CACHE:
        _CACHE[key] = build(S, list(layers))
    nc = _CACHE[key]
    cfull = make_consts()
    cst = np.ascontiguousarray(np.concatenate([cfull[:, 0:256], cfull[:, 1536:1537]], axis=1))
    cst2 = np.ascontiguousarray(cfull[:, 256:1536])
    in_maps = []
    for b in range(Bn):
        m = {"x": np.ascontiguousarray(x[b]), "c": np.ascontiguousarray(c[b:b + 1]),
             "positions": np.ascontiguousarray(positions[b:b + 1]).astype(np.int32), "cst": cst, "cst2": cst2}
        for n in _W_NAMES:
            m[n] = weights[n]
        in_maps.append(m)
    if runner is None:
        res = run_bass_kernel_spmd(nc, in_maps, core_ids=list(range(Bn)))
    else:
        res = runner(nc, in_maps)
    return np.stack([res.results[b]["y"] for b in range(Bn)], axis=0)


LAUNCH_GROUPS = [[0, 1, 2, 3]]


def kernel(**inputs):
    x = np.asarray(inputs["x"], dtype=np.float32)
    c = np.asarray(inputs["c"], dtype=np.float32)
    positions = np.asarray(inputs["positions"])
    weights = {n: np.ascontiguousarray(np.asarray(inputs[n], dtype=np.float32)) for n in _W_NAMES}
    for grp in LAUNCH_GROUPS:
        x = run_layers(x, c, positions, weights, grp)
    return x.astype(np.float32)
```
